# Optimizing a Trainium2 kernel written in Bass

```python
import math
import jax, jax.numpy as jnp
from jax import lax
import numpy as np

D_MODEL = 1024
BATCH = 8
SEQ = 2048
DEPTH = 1

MEM_LEN = 256
HG_HEADS = 4
HG_DIM = 128
HG_WIDTH = HG_HEADS * HG_DIM
HG_CHUNK = 64
DA_HEADS = 4
DA_QK_DIM = 64
DA_V_DIM = 2 * DA_QK_DIM
DA_QK_WIDTH = DA_HEADS * 2 * DA_QK_DIM
DA_WIDTH = DA_HEADS * DA_V_DIM
ROPE_DIM = DA_QK_DIM // 4
ROPE_THETA = 500000.0
Q_BLOCK = 128
MIX_WIDTH = HG_WIDTH + DA_WIDTH
XA_HEADS = 4
XA_DIM = D_MODEL // XA_HEADS
N_EXPERTS = 16
EC_FACTOR = 2
D_FF_EXPERT = 2 * D_MODEL
LN_EPS = 1e-5
RMS_EPS = 1e-6
DEEPNORM_ALPHA = (2.0 * DEPTH) ** 0.25
DEEPNORM_BETA = (8.0 * DEPTH) ** -0.25
IN_SIZES = (HG_WIDTH, HG_WIDTH, HG_WIDTH, HG_WIDTH, HG_WIDTH, DA_QK_WIDTH, DA_QK_WIDTH, DA_WIDTH)
IN_COLS = sum(IN_SIZES)
IN_OFFSETS = tuple(sum(IN_SIZES[:i + 1]) for i in range(len(IN_SIZES) - 1))

kernel_name = 'hybrid_hgrn2_diffattn_ecmoe_encoder'


def layer_norm(x, g, b):
    xf = x.astype(jnp.float32)
    mu = jnp.mean(xf, axis=-1, keepdims=True)
    var = jnp.mean(jnp.square(xf - mu), axis=-1, keepdims=True)
    y = (xf - mu) * lax.rsqrt(var + LN_EPS) * g.astype(jnp.float32) + b.astype(jnp.float32)
    return y.astype(x.dtype)


def rms_norm(x, g):
    xf = x.astype(jnp.float32)
    y = xf * lax.rsqrt(jnp.mean(jnp.square(xf), axis=-1, keepdims=True) + RMS_EPS) * g.astype(jnp.float32)
    return y.astype(x.dtype)


def rope_tables(seq_len):
    inv = 1.0 / (ROPE_THETA ** (jnp.arange(0, ROPE_DIM, 2, dtype=jnp.float32) / ROPE_DIM))
    ang = jnp.arange(seq_len, dtype=jnp.float32)[:, None] * inv[None, :]
    return jnp.cos(ang), jnp.sin(ang)


def apply_partial_rope(t, cos, sin):
    rot, rest = t[..., :ROPE_DIM], t[..., ROPE_DIM:]
    r1, r2 = rot[..., :ROPE_DIM // 2], rot[..., ROPE_DIM // 2:]
    c = cos.astype(t.dtype)
    s = sin.astype(t.dtype)
    return jnp.concatenate([r1 * c - r2 * s, r2 * c + r1 * s, rest], axis=-1)


def gla_chunkwise(q, k, v, log_f):
    out_dtype = v.dtype
    B, H, L, dk = q.shape
    dv = v.shape[-1]
    n = L // HG_CHUNK
    rs = lambda t: t.astype(jnp.float32).reshape(B, H, n, HG_CHUNK, t.shape[-1])
    q, k, v, log_f = rs(q), rs(k), rs(v), rs(log_f)
    bcum = jnp.cumsum(log_f, axis=3)
    b_last = bcum[:, :, :, -1:, :]
    q_in = q * jnp.exp(bcum)
    k_in = k * jnp.exp(-bcum)
    k_out = k * jnp.exp(b_last - bcum)
    mask = jnp.tril(jnp.ones((HG_CHUNK, HG_CHUNK), dtype=bool))
    att = jnp.where(mask, jnp.einsum('bhnck,bhnsk->bhncs', q_in, k_in), 0.0)
    o_intra = jnp.einsum('bhncs,bhnsv->bhncv', att, v)
    delta = jnp.einsum('bhnck,bhncv->nbhkv', k_out, v)
    decay = jnp.moveaxis(jnp.exp(b_last[:, :, :, 0, :]), 2, 0)

    def step(S, inp):
        d, dl = inp
        return d[..., None] * S + dl, S

    S0 = jnp.zeros((B, H, dk, dv), jnp.float32)
    _, S_prev = lax.scan(step, S0, (decay, delta))
    o_inter = jnp.einsum('bhnck,nbhkv->bhncv', q_in, S_prev)
    return (o_intra + o_inter).reshape(B, H, L, dv).astype(out_dtype)


def hgrn2_group(u_q, u_i, u_g, u_ff, u_fb, lb_fwd, lb_bwd, norm_g):
    B, L, _ = u_q.shape
    heads = lambda t: t.reshape(B, L, HG_HEADS, HG_DIM).transpose(0, 2, 1, 3)
    q = heads(jax.nn.silu(u_q))
    v = heads(u_i)

    def gates(u_f, lb):
        f = lb + (1.0 - lb) * jax.nn.sigmoid(u_f.astype(jnp.float32))
        return heads((1.0 - f).astype(u_f.dtype)), heads(jnp.log(f))

    k_f, lf_f = gates(u_ff, lb_fwd)
    k_b, lf_b = gates(u_fb, lb_bwd)
    flip = lambda t: jnp.flip(t, axis=2)
    o_f = gla_chunkwise(q, k_f, v, lf_f)
    o_b = flip(gla_chunkwise(flip(q), flip(k_b), flip(v), flip(lf_b)))
    o = (o_f + o_b).transpose(0, 2, 1, 3)
    o = rms_norm(o, norm_g.reshape(HG_HEADS, HG_DIM))
    return o.reshape(B, L, HG_WIDTH) * jax.nn.silu(u_g)


def diff_attention_group(u_q, u_k, u_v, lam, lambda_init, subln_g, cos, sin):
    B, L, _ = u_q.shape
    q = u_q.reshape(B, L, 2 * DA_HEADS, DA_QK_DIM).transpose(0, 2, 1, 3)
    k = u_k.reshape(B, L, 2 * DA_HEADS, DA_QK_DIM).transpose(0, 2, 1, 3)
    v = u_v.reshape(B, L, DA_HEADS, DA_V_DIM).transpose(0, 2, 1, 3)
    q = apply_partial_rope(q, cos, sin) * (DA_QK_DIM ** -0.5)
    k = apply_partial_rope(k, cos, sin)
    nb = L // Q_BLOCK
    qb = jnp.moveaxis(q.reshape(B, 2 * DA_HEADS, nb, Q_BLOCK, DA_QK_DIM), 2, 0)

    def block(q_blk):
        s = jnp.einsum('bgqd,bgkd->bgqk', q_blk, k).astype(jnp.float32)
        p = jax.nn.softmax(s, axis=-1).reshape(B, DA_HEADS, 2, Q_BLOCK, L)
        a = (p[:, :, 0] - lam * p[:, :, 1]).astype(v.dtype)
        return jnp.einsum('bhqk,bhkv->bhqv', a, v)

    o = lax.map(block, qb)
    o = jnp.moveaxis(o, 0, 2).reshape(B, DA_HEADS, L, DA_V_DIM)
    o = rms_norm(o, subln_g) * (1.0 - lambda_init)
    return o.transpose(0, 2, 1, 3).reshape(B, L, DA_WIDTH)


def memory_cross_attention(h, mem, wq, wk, wv, wo):
    B, L, _ = h.shape
    M = mem.shape[1]
    q = jnp.einsum('bld,de->ble', h, wq).reshape(B, L, XA_HEADS, XA_DIM)
    k = jnp.einsum('bmd,de->bme', mem, wk).reshape(B, M, XA_HEADS, XA_DIM)
    v = jnp.einsum('bmd,de->bme', mem, wv).reshape(B, M, XA_HEADS, XA_DIM)
    s = jnp.einsum('blhd,bmhd->bhlm', q, k).astype(jnp.float32) * (XA_DIM ** -0.5)
    p = jax.nn.softmax(s, axis=-1).astype(v.dtype)
    o = jnp.einsum('bhlm,bmhd->blhd', p, v).reshape(B, L, D_MODEL)
    return jnp.einsum('bld,de->ble', o, wo)


def expert_choice_moe(h, w_router, w_gate, w_up, w_down):
    B, L, D = h.shape
    cap = EC_FACTOR * L // N_EXPERTS
    aff = jax.nn.softmax(jnp.einsum('bld,de->ble', h, w_router).astype(jnp.float32), axis=-1)
    gates, idx = lax.top_k(jnp.swapaxes(aff, 1, 2), cap)
    xs = jax.vmap(lambda hb, ib: hb[ib])(h, idx)
    a = jax.nn.silu(jnp.einsum('becd,edf->becf', xs, w_gate)) * jnp.einsum('becd,edf->becf', xs, w_up)
    y = jnp.einsum('becf,efd->becd', a, w_down) * gates[..., None].astype(h.dtype)
    return jax.vmap(lambda ib, yb: jnp.zeros((L, D), yb.dtype).at[ib.reshape(-1)].add(yb.reshape(-1, D)))(idx, y)


def setup_inputs(seed: int = 0) -> dict:
    key = jax.random.key(seed)
    ks = jax.random.split(key, 32)
    f32 = jnp.float32
    nrm = lambda k, shape, scale: jax.random.normal(k, shape, f32) * scale
    gain = lambda k, shape: 1.0 + 0.02 * jax.random.normal(k, shape, f32)
    return {
        'x': nrm(ks[0], (BATCH, SEQ, D_MODEL), 1.0),
        'mem': nrm(ks[1], (BATCH, MEM_LEN, D_MODEL), 1.0),
        'emb_ln_g': gain(ks[2], (D_MODEL,)),
        'emb_ln_b': nrm(ks[3], (D_MODEL,), 0.02),
        'w_in': nrm(ks[4], (DEPTH, D_MODEL, IN_COLS), D_MODEL ** -0.5),
        'hg_lb_logits': nrm(ks[5], (2, DEPTH + 1, HG_WIDTH), 0.1),
        'hg_norm_g': gain(ks[6], (DEPTH, HG_WIDTH)),
        'da_lambda_q1': nrm(ks[7], (DEPTH, DA_QK_DIM), 0.1),
        'da_lambda_k1': nrm(ks[8], (DEPTH, DA_QK_DIM), 0.1),
        'da_lambda_q2': nrm(ks[9], (DEPTH, DA_QK_DIM), 0.1),
        'da_lambda_k2': nrm(ks[10], (DEPTH, DA_QK_DIM), 0.1),
        'da_subln_g': gain(ks[11], (DEPTH, DA_V_DIM)),
        'w_mix_out': nrm(ks[12], (DEPTH, MIX_WIDTH, D_MODEL), MIX_WIDTH ** -0.5 * DEEPNORM_BETA),
        'ln1_g': gain(ks[13], (DEPTH, D_MODEL)),
        'ln1_b': nrm(ks[14], (DEPTH, D_MODEL), 0.02),
        'xa_wq': nrm(ks[15], (DEPTH, D_MODEL, D_MODEL), D_MODEL ** -0.5),
        'xa_wk': nrm(ks[16], (DEPTH, D_MODEL, D_MODEL), D_MODEL ** -0.5),
        'xa_wv': nrm(ks[17], (DEPTH, D_MODEL, D_MODEL), D_MODEL ** -0.5),
        'xa_wo': nrm(ks[18], (DEPTH, D_MODEL, D_MODEL), D_MODEL ** -0.5 * DEEPNORM_BETA),
        'ln2_g': gain(ks[19], (DEPTH, D_MODEL)),
        'ln2_b': nrm(ks[20], (DEPTH, D_MODEL), 0.02),
        'w_router': nrm(ks[21], (DEPTH, D_MODEL, N_EXPERTS), D_MODEL ** -0.5),
        'w_gate': nrm(ks[22], (DEPTH, N_EXPERTS, D_MODEL, D_FF_EXPERT), D_MODEL ** -0.5),
        'w_up': nrm(ks[23], (DEPTH, N_EXPERTS, D_MODEL, D_FF_EXPERT), D_MODEL ** -0.5),
        'w_down': nrm(ks[24], (DEPTH, N_EXPERTS, D_FF_EXPERT, D_MODEL), D_FF_EXPERT ** -0.5 * DEEPNORM_BETA),
        'ln3_g': gain(ks[25], (DEPTH, D_MODEL)),
        'ln3_b': nrm(ks[26], (DEPTH, D_MODEL), 0.02),
    }


def reference(x, mem, emb_ln_g, emb_ln_b, w_in, hg_lb_logits, hg_norm_g, da_lambda_q1, da_lambda_k1,
              da_lambda_q2, da_lambda_k2, da_subln_g, w_mix_out, ln1_g, ln1_b, xa_wq, xa_wk, xa_wv, xa_wo,
              ln2_g, ln2_b, w_router, w_gate, w_up, w_down, ln3_g, ln3_b):
    L = x.shape[1]
    cos, sin = rope_tables(L)
    lb_all = jnp.cumsum(jax.nn.softmax(hg_lb_logits.astype(jnp.float32), axis=1), axis=1)
    h = layer_norm(x, emb_ln_g, emb_ln_b)
    for l in range(DEPTH):
        lambda_init = 0.8 - 0.6 * math.exp(-0.3 * l)
        u = jnp.einsum('bld,dc->blc', h, w_in[l])
        hq, hi, hgt, hff, hfb, dq, dk, dv = jnp.split(u, IN_OFFSETS, axis=-1)
        hg_out = hgrn2_group(hq, hi, hgt, hff, hfb, lb_all[0, l], lb_all[1, l], hg_norm_g[l])
        lam = (jnp.exp(jnp.sum(da_lambda_q1[l].astype(jnp.float32) * da_lambda_k1[l].astype(jnp.float32)))
               - jnp.exp(jnp.sum(da_lambda_q2[l].astype(jnp.float32) * da_lambda_k2[l].astype(jnp.float32)))
               + lambda_init)
        da_out = diff_attention_group(dq, dk, dv, lam, lambda_init, da_subln_g[l], cos, sin)
        mix = jnp.einsum('blc,cd->bld', jnp.concatenate([hg_out, da_out], axis=-1), w_mix_out[l])
        h = layer_norm(DEEPNORM_ALPHA * h + mix, ln1_g[l], ln1_b[l])
        xa = memory_cross_attention(h, mem, xa_wq[l], xa_wk[l], xa_wv[l], xa_wo[l])
        h = layer_norm(DEEPNORM_ALPHA * h + xa, ln2_g[l], ln2_b[l])
        moe = expert_choice_moe(h, w_router[l], w_gate[l], w_up[l], w_down[l])
        h = layer_norm(DEEPNORM_ALPHA * h + moe, ln3_g[l], ln3_b[l])
    return h
```

```python
import contextlib
import math
import numpy as np
import concourse.bass as bass
import concourse.mybir as mybir
from concourse.bass_utils import run_bass_kernel_spmd

F32 = mybir.dt.float32
F32R = mybir.dt.float32r
AF = mybir.ActivationFunctionType
ALU = mybir.AluOpType
AX = mybir.AxisListType

T = 2048
D = 1024
ALPHA = 2.0 ** 0.25
LN_EPS = 1e-5
RMS_EPS = 1e-6
LAMBDA_INIT = 0.8 - 0.6 * math.exp(0.0)
CAP = 256
NITER = 28
import os
_DBG_SKIP64 = os.environ.get('DBG_SKIP64') == '1'
ENGS = ["pe", "act", "dve", "pool", "sp"]


class Op:
    __slots__ = ("i", "eng", "fn", "deps", "dma", "out")

    def __init__(self, i, eng, fn, deps, dma, out):
        self.i, self.eng, self.fn, self.deps, self.dma, self.out = i, eng, fn, deps, dma, out


class Prog:
    def __init__(self):
        self.ops = []
        self.lastw = {}
        self.readers = {}
        self.since_barrier = []
        self.barrier_op = None

    def add(self, eng, fn, r=(), w=(), dma=False, out=False):
        i = len(self.ops)
        deps = set()
        if self.barrier_op is not None:
            deps.add(self.barrier_op)
        for t in r:
            if t in self.lastw:
                deps.add(self.lastw[t])
        for t in w:
            if t in self.lastw:
                deps.add(self.lastw[t])
            deps.update(self.readers.get(t, ()))
        for t in r:
            self.readers.setdefault(t, []).append(i)
        for t in w:
            self.lastw[t] = i
            self.readers[t] = []
        deps.discard(i)
        self.ops.append(Op(i, eng, fn, deps, dma, out))
        self.since_barrier.append(i)
        return i

    def barrier(self):
        i = len(self.ops)
        deps = set(self.since_barrier)
        self.ops.append(Op(i, "pool", lambda e: e.nop(), deps, False, False))
        self.since_barrier = []
        self.barrier_op = i

    def emit(self, nc, n_dma_sems=10):
        ops = self.ops
        n = len(ops)
        needs = [False] * n
        for op in ops:
            for d in op.deps:
                needs[d] = True
        with contextlib.ExitStack() as st:
            csem = {e: st.enter_context(nc.semaphore("c_" + e)) for e in ENGS}
            dsems = {e: [st.enter_context(nc.semaphore("d_%s%d" % (e, k))) for k in range(n_dma_sems)]
                     for e in ("sp", "act", "pool")}
            duse = {e: [0] * n_dma_sems for e in dsems}
            drr = {e: 0 for e in dsems}
            cnt = {e: 0 for e in ENGS}
            sig = [None] * n
            vc = [None] * n
            engvc = {e: {} for e in ENGS}
            dknown = {e: set() for e in ENGS}
            plan = {e: [] for e in ENGS}
            out_dmas = []
            for op in ops:
                e = op.eng
                my = engvc[e]
                waits = []
                for d in sorted(op.deps):
                    dop = ops[d]
                    if dop.dma:
                        if d in dknown[e]:
                            continue
                        dknown[e].add(d)
                        waits.append(sig[d])
                    else:
                        if dop.eng == "pe" and e == "pe":
                            continue
                        c = sig[d][1]
                        if my.get(dop.eng, 0) >= c:
                            continue
                        waits.append((csem[dop.eng], c))
                    for k, v in vc[d].items():
                        if my.get(k, 0) < v:
                            my[k] = v
                if op.dma:
                    k = drr[e]
                    drr[e] = (k + 1) % n_dma_sems
                    s = dsems[e][k]
                    if duse[e][k] > 0:
                        waits.append((s, 16 * duse[e][k]))
                    duse[e][k] += 1
                    sig[op.i] = (s, 16 * duse[e][k])
                    vc[op.i] = dict(my)
                    plan[e].append((waits, op, (s, 16)))
                    if op.out:
                        out_dmas.append(sig[op.i])
                else:
                    if needs[op.i]:
                        cnt[e] += 1
                        sig[op.i] = (csem[e], cnt[e])
                        v = dict(my)
                        v[e] = cnt[e]
                        vc[op.i] = v
                        plan[e].append((waits, op, (csem[e], 1)))
                    else:
                        plan[e].append((waits, op, None))
            self.stats = {e: len(plan[e]) for e in ENGS}

            def run(eng, e):
                for waits, op, s in plan[e]:
                    for sem, val in waits:
                        eng.wait_ge(sem, val)
                    ins = op.fn(eng)
                    if s is not None:
                        ins.then_inc(s[0], s[1])
                if e == "sp":
                    for sem, val in out_dmas:
                        eng.wait_ge(sem, val)

            with nc.Block() as block:
                @block.tensor
                def _(eng):
                    run(eng, "pe")

                @block.scalar
                def _(eng):
                    run(eng, "act")

                @block.vector
                def _(eng):
                    run(eng, "dve")

                @block.gpsimd
                def _(eng):
                    run(eng, "pool")

                @block.sync
                def _(eng):
                    run(eng, "sp")


def tk(name, lo, hi):
    return ["%s.%d" % (name, i) for i in range(lo, hi)]


def build(stop=None):
    nc = bass.Bass("TRN2", target_bir_lowering=False)
    P = Prog()

    def din(name, shape):
        return nc.dram_tensor(name, list(shape), F32, kind="ExternalInput").ap()

    x_d = din("x", [T, D])
    mem_d = din("mem", [256, D])
    w_in_d = din("w_in", [D, 4096])
    w_sw_d = din("w_sw", [D, 1024])
    w_mix_d = din("w_mix", [D, D])
    wq_d = din("xa_wq", [D, D])
    wk_d = din("xa_wk", [D, D])
    wv_d = din("xa_wv", [D, D])
    wo_d = din("xa_wo", [D, D])
    wr_d = din("w_router", [D, 16])
    if stop is None or stop >= 6:
        wg_d = din("w_gate", [16, 16, 128, 1024])
        wu_d = din("w_up", [16, 16, 128, 1024])
        wd_d = din("w_down", [16, 2048, D])
    lng_d = din("ln_g", [4, D])
    lnb_d = din("ln_b", [4, D])
    lb_d = din("hg_lb", [16, 128])
    hgn_d = din("hg_norm_g", [4, 128])
    sub_d = din("da_subln_g", [1, 128])
    lam_d = din("da_lam", [4, 64])
    c_ident = din("c_ident", [128, 128])
    c_aI = din("c_aI", [128, 128])
    c_aI4 = din("c_aI4", [128, 2048])
    c_ones = din("c_ones", [128, 128])
    c_triF = din("c_triF", [128, 128])
    c_triB = din("c_triB", [128, 128])
    c_rmask = din("c_rmask", [128, 512])
    c_ropeC = din("c_ropeC", [128, T])
    c_ropeS = din("c_ropeS", [128, T])
    c_iotaf = din("c_iotaf", [128, 256])
    out_d = nc.dram_tensor("out", [T, D], F32, kind="ExternalOutput").ap()
    dbg = stop is not None
    if dbg:
        dbgA_d = nc.dram_tensor("dbgA", [128, 16384], F32, kind="ExternalOutput").ap()
        dbgB_d = nc.dram_tensor("dbgB", [128, 16384], F32, kind="ExternalOutput").ap()

    st = contextlib.ExitStack()
    with st:
        def sb(name, shape, dt=F32):
            return st.enter_context(nc.sbuf_tensor(name, list(shape), dt))

        RA = sb("RA", [128, 16384], F32R)
        RB = sb("RB", [128, 16384], F32R)
        SR = sb("SR", [128, 12288], F32R)
        FA = sb("FA", [128, 6400], F32)

        def Rb(off, n):
            return SR[:, off:off + n]

        def Fb(off, n):
            return FA[:, off:off + n]
        smallp = sb("smallp", [128, 768], F32)
        ident = sb("ident", [128, 128])
        aI = sb("aI", [128, 128])
        onesR = sb("onesR", [128, 128], F32R)
        gB = FA[:, 2048:3072]
        bB = FA[:, 3072:4096]
        xt = [FA[:, i * 1024:(i + 1) * 1024] for i in range(2)]
        stt = sb("stt", [128, 32])
        cols = sb("cols", [128, 64])
        ps = [st.enter_context(nc.psum_tensor("ps%d" % i, [128, 512], F32)) for i in range(8)]

        hT = RA[:, :].rearrange("p (j t) -> p j t", t=T)
        catT = RB[:, :].rearrange("p (j t) -> p j t", t=T)

        def dma(q, out, in_, r=(), w=(), outp=False):
            return P.add(q, lambda e: e.dma_start(out=out, in_=in_), r, w, dma=True, out=outp)

        def pe(mms, r, w, skip=False):
            def f(e):
                for (o, l, rh, s, t) in mms:
                    ins = e.matmul(o, l, rh, start=s, stop=t, skip_group_check=skip)
                return ins
            return P.add("pe", f, r, w)

        def petr(trs, r, w):
            def f(e):
                for (o, i_, idn) in trs:
                    ins = e.transpose(o, i_, idn)
                return ins
            return P.add("pe", f, r, w)

        def A(fn, r, w):
            return P.add("act", fn, r, w)

        def V(fn, r, w):
            return P.add("dve", fn, r, w)

        _alt = [0]

        def copy_any(out, in_, r, w):
            _alt[0] ^= 1
            if _alt[0]:
                return A(lambda e: e.copy(out, in_), r, w)
            return V(lambda e: e.tensor_copy(out, in_), r, w)

        if dbg:
            V(lambda e: e.memset(RA[:, :], 0.0), [], tk("hT", 0, 16))
            V(lambda e: e.memset(RB[:, :], 0.0), [], ["cat%d.%d" % (k, q) for k in range(8) for q in range(4)])
        dma("sp", ident[:, :], c_ident, w=["ident"])
        dma("sp", aI[:, :], c_aI, w=["aI"])
        dma("pool", onesR[:, :], c_ones, w=["onesR"])

        def load_ln(k):
            dma("sp", gB, lng_d[k].partition_broadcast(128), w=["gB"])
            dma("sp", bB, lnb_d[k].partition_broadcast(128), w=["bB"])

        def ln_chunk_gen(src_lo, src_hi, rtok, dst, dtok, tmp, ttok, par=0):
            so = 16 * par
            sx = "%d" % par
            s6 = stt[:, so:so + 12].rearrange("p (a b) -> p a b", b=6)
            mean, var, rstd, nmr = (stt[:, so + 12:so + 13], stt[:, so + 13:so + 14], stt[:, so + 14:so + 15], stt[:, so + 15:so + 16])
            V(lambda e: e.bn_stats(s6[:, 0, :], src_lo), rtok, ["st6a" + sx])
            yield
            V(lambda e: e.bn_stats(s6[:, 1, :], src_hi), rtok, ["st6b" + sx])
            yield
            V(lambda e: e.bn_aggr(stt[:, so + 12:so + 14], stt[:, so:so + 12]), ["st6a" + sx, "st6b" + sx], ["mv" + sx])
            yield
            V(lambda e: e.tensor_scalar(rstd, var, LN_EPS, None, ALU.add), ["mv" + sx], ["rstd" + sx])
            yield
            A(lambda e: e.activation(rstd, rstd, AF.Ln), ["rstd" + sx], ["rstd" + sx])
            yield
            A(lambda e: e.activation(rstd, rstd, AF.Exp, scale=-0.5), ["rstd" + sx], ["rstd" + sx])
            yield
            V(lambda e: e.scalar_tensor_tensor(nmr, mean, -1.0, rstd, ALU.mult, ALU.mult), ["mv" + sx, "rstd" + sx], ["nmr" + sx])
            yield
            A(lambda e: e.activation(tmp[:, 0:512], src_lo, AF.Identity, bias=nmr, scale=rstd),
              rtok + ["rstd" + sx, "nmr" + sx], [ttok + "a"] + list(dtok))
            yield
            A(lambda e: e.activation(tmp[:, 512:1024], src_hi, AF.Identity, bias=nmr, scale=rstd),
              rtok + ["rstd" + sx, "nmr" + sx], [ttok + "b"] + list(dtok))
            yield
            V(lambda e: e.tensor_tensor(tmp, tmp, gB, ALU.mult), [ttok + "a", ttok + "b", "gB"], [ttok + "a", ttok + "b"])
            yield
            V(lambda e: e.tensor_tensor(dst, tmp, bB, ALU.add), [ttok + "a", ttok + "b", "bB"], dtok)
            yield

        def ln_chunk(src_lo, src_hi, rtok, dst, dtok, tmp, ttok, key):
            for _ in ln_chunk_gen(src_lo, src_hi, rtok, dst, dtok, tmp, ttok, 0):
                pass

        def interleave(*gens):
            gens = [g for g in gens if g is not None]
            while gens:
                for g in list(gens):
                    try:
                        next(g)
                    except StopIteration:
                        gens.remove(g)

        def tm_to_fm(src, stok, tc, pbanks):
            for b in range(2):
                pb = pbanks[b]
                o3 = ps[pb][:, :].rearrange("p (a c) -> p a c", c=128)
                petr([(o3[:, a, :], src[:, (4 * b + a) * 128:(4 * b + a + 1) * 128], ident[:, :]) for a in range(4)],
                     stok + ["ident"], ["ps.%d" % pb])
                copy_any(hT[:, 4 * b:4 * b + 4, tc * 128:(tc + 1) * 128], o3, ["ps.%d" % pb], ["hT.%d" % tc])

        def dump_and_finish():
            dma("sp", dbgA_d, RA[:, :].bitcast(F32), r=["DUMP"], outp=True)
            dma("sp", dbgB_d, RB[:, :].bitcast(F32), r=["DUMP"], outp=True)

        load_ln(0)
        def ln0_chain(tc):
            par = tc % 2
            xb = xt[par]
            xk = "xt%d" % par
            dma("sp", xb[:, :], x_d[tc * 128:(tc + 1) * 128, :], w=[xk])
            yield
            yield from ln_chunk_gen(xb[:, 0:512], xb[:, 512:1024], [xk], xb[:, :], [xk], xb[:, :], xk, par)
            tm_to_fm(xb, [xk], tc, (6, 7) if par == 0 else (4, 5))
            yield

        for tc in range(0, 16, 2):
            interleave(ln0_chain(tc), ln0_chain(tc + 1))

        lbt = FA[0:16, 4096:4224]
        dma("sp", lbt, lb_d, w=["lbt"])
        petr([(ps[0][:, 0:16], lbt, ident[0:16, 0:16])], ["lbt", "ident"], ["ps.0"])
        V(lambda e: e.tensor_copy(cols[:, 48:64], ps[0][:, 0:16]), ["ps.0"], ["c48"])
        psl = cols[:, 48:64].rearrange("p (a l h) -> p a l h", a=2, l=2)
        c_lb = cols[:, 0:8].rearrange("p (a h) -> p a h", a=2)
        c_oml = cols[:, 8:16].rearrange("p (a h) -> p a h", a=2)
        V(lambda e: e.tensor_tensor(c_lb, psl[:, :, 1, :], psl[:, :, 0, :], ALU.subtract), ["c48"], ["c_lb"])
        A(lambda e: e.activation(cols[:, 0:8], cols[:, 0:8], AF.Exp), ["c_lb"], ["c_lb"])
        V(lambda e: e.tensor_scalar(cols[:, 0:8], cols[:, 0:8], 1.0, None, ALU.add), ["c_lb"], ["c_lb"])
        V(lambda e: e.reciprocal(cols[:, 0:8], cols[:, 0:8]), ["c_lb"], ["c_lb"])
        V(lambda e: e.tensor_scalar(cols[:, 8:16], cols[:, 0:8], -1.0, 1.0, ALU.mult, ALU.add), ["c_lb"], ["c_oml"])
        V(lambda e: e.tensor_scalar(cols[:, 40:48], cols[:, 8:16], 0.5, None, ALU.mult), ["c_oml"], ["c_oml"])
        V(lambda e: e.tensor_tensor(cols[:, 24:32], cols[:, 0:8], cols[:, 40:48], ALU.add), ["c_lb", "c_oml"], ["c_lb"])
        gt = FA[0:4, 4224:4352]
        dma("sp", gt, hgn_d, w=["gt"])
        petr([(ps[1][:, 0:4], gt, ident[0:4, 0:4])], ["gt", "ident"], ["ps.1"])
        V(lambda e: e.tensor_scalar(cols[:, 16:20], ps[1][:, 0:4], math.sqrt(128.0), None, ALU.mult), ["ps.1"], ["c_hgn"])
        sg1 = FA[0:1, 4352:4480]
        dma("sp", sg1, sub_d, w=["sg1"])
        petr([(ps[2][:, 0:1], sg1, ident[0:1, 0:1])], ["sg1", "ident"], ["ps.2"])
        V(lambda e: e.tensor_scalar(cols[:, 20:21], ps[2][:, 0:1], math.sqrt(128.0) * (1.0 - LAMBDA_INIT), None, ALU.mult),
          ["ps.2"], ["c_sub"])
        lmb = FA[:, 4480:4736].rearrange("p (a b) -> p a b", b=64)
        for a in range(4):
            dma("sp", lmb[:, a, :], lam_d[a].partition_broadcast(128), w=["lmb%d" % a])
        V(lambda e: e.tensor_tensor(lmb[:, 0, :], lmb[:, 0, :], lmb[:, 1, :], ALU.mult), ["lmb0", "lmb1"], ["lmb0"])
        V(lambda e: e.tensor_tensor(lmb[:, 2, :], lmb[:, 2, :], lmb[:, 3, :], ALU.mult), ["lmb2", "lmb3"], ["lmb2"])
        V(lambda e: e.reduce_sum(cols[:, 22:23], lmb[:, 0, :], axis=AX.X), ["lmb0"], ["c22"])
        V(lambda e: e.reduce_sum(cols[:, 23:24], lmb[:, 2, :], axis=AX.X), ["lmb2"], ["c23"])
        A(lambda e: e.activation(cols[:, 22:24], cols[:, 22:24], AF.Exp), ["c22", "c23"], ["c22", "c23"])
        V(lambda e: e.tensor_tensor(cols[:, 21:22], cols[:, 23:24], cols[:, 22:23], ALU.subtract), ["c22", "c23"], ["c_nlam"])
        V(lambda e: e.tensor_scalar(cols[:, 21:22], cols[:, 21:22], -LAMBDA_INIT, None, ALU.add), ["c_nlam"], ["c_nlam"])
        P.barrier()

        if stop == 0:
            P.add("pool", lambda e: e.nop(), [], ["DUMP"])
            dump_and_finish()
            P.emit(nc)
            return nc

        wctr = [0]
        ring = {"base": 3584, "n": 7}

        def wslice(w_d, c0):
            k = wctr[0] % ring["n"]
            wctr[0] += 1
            o_ = ring["base"] + k * 1024
            v = SR[:, o_:o_ + 1024].rearrange("p (j c) -> p j c", c=128)
            dma("pool", v, w_d.rearrange("(j p) c -> p j c", p=128)[:, :, c0:c0 + 128], w=["WB.%d" % k])
            return v, "WB.%d" % k

        def projB(wv_, wtok, tb, pb):
            pe([(ps[pb][:, :], wv_[:, j, :], hT[:, j, tb * 512:(tb + 1) * 512], j == 0, j == 7) for j in range(8)],
               [wtok] + tk("hT", tb * 4, tb * 4 + 4), ["ps.%d" % pb])


        triF = Fb(4096, 128)
        triB = Fb(4224, 128)
        rmask = Fb(4352, 512)
        dma("sp", triF, c_triF, w=["triF"])
        dma("sp", triB, c_triB, w=["triB"])
        dma("sp", rmask, c_rmask, w=["rmask"])
        qs, fT, kT, Pc, eA = [Fb(i * 512, 512) for i in range(5)]
        Sf = [Fb(4864, 1152), Fb(2560, 1152)]
        ring["base"], ring["n"] = 7168, 5
        pt = "b0"

        def rbuf(par, off, n=512):
            return Rb(par * 3584 + off, n)

        blocks = [(hh, di, bi, tb) for hh in range(4) for di in range(2)
                  for bi, tb in enumerate([0, 1, 2, 3] if di == 0 else [3, 2, 1, 0])]
        ctxs = {}
        wcur = {}

        def projs(k):
            hh, di, bi, tb = blocks[k]
            if bi == 0:
                wcur[(hh, di)] = (wslice(w_in_d, 0 + hh * 128), wslice(w_in_d, 512 + hh * 128),
                                  wslice(w_in_d, (1536 if di == 0 else 2048) + hh * 128))
            (wq_v, wq_t), (wi_v, wi_t), (wf_v, wf_t) = wcur[(hh, di)]
            projB(wq_v, wq_t, tb, 6)
            projB(wf_v, wf_t, tb, 7)
            projB(wi_v, wi_t, tb, k % 2)

        def stageA1(k):
            hh, di, bi, tb = blocks[k]
            par = k % 2
            rt = "r%d" % par
            lbc = cols[:, 24 + di * 4 + hh:24 + di * 4 + hh + 1]
            omlc = cols[:, 40 + di * 4 + hh:40 + di * 4 + hh + 1]
            qin, kin = rbuf(par, 0), rbuf(par, 512)
            koTM = rbuf(par, 1024).rearrange("p (a c) -> p a c", c=128)
            att = rbuf(par, 1536)
            vTM = rbuf(par, 2048).rearrange("p (a c) -> p a c", c=128)
            SentF = rbuf(par, 2560, 1024).rearrange("p (i c) -> p i c", c=128)
            dec = cols[:, 32:40] if par == 0 else cols[:, 56:64]
            ctxs[k] = dict(qin=qin, kin=kin, koTM=koTM, att=att, vTM=vTM, SentF=SentF, dec=dec)
            A(lambda e: e.activation(qs, ps[6][:, :], AF.Silu), ["ps.6"], [pt + "qs"])
            yield
            A(lambda e: e.activation(fT, ps[7][:, :], AF.Tanh, scale=0.5), ["ps.7"], [pt + "fT"])
            yield
            V(lambda e: e.tensor_scalar(fT, fT, omlc, lbc, ALU.mult, ALU.add), [pt + "fT", "c_lb", "c_oml"], [pt + "fT"])
            yield
            V(lambda e: e.tensor_scalar(kT, fT, -1.0, 1.0, ALU.mult, ALU.add), [pt + "fT"], [pt + "kT"])
            yield
            A(lambda e: e.activation(fT, fT, AF.Ln), [pt + "fT"], [pt + "fT"])
            yield
            V(lambda e: e.tensor_tensor_scan(Pc, rmask, fT, 0.0, ALU.mult, ALU.add), [pt + "fT", "rmask"], [pt + "Pc"])
            yield
            P3 = Pc.rearrange("p (n c) -> p n c", c=64)
            f3 = fT.rearrange("p (n c) -> p n c", c=64)
            tot = P3[:, :, 63:64]
            totb = tot.broadcast_to([128, 8, 64])
            A(lambda e: e.activation(dec.rearrange("p (n o) -> p n o", o=1), tot, AF.Exp), [pt + "Pc"], [rt + "dec"])
            yield
            eA3 = eA.rearrange("p (n c) -> p n c", c=64)
            if di == 0:
                A(lambda e: e.activation(eA, Pc, AF.Exp), [pt + "Pc"], [pt + "eA"])
                yield
                V(lambda e: e.tensor_tensor(qin, qs, eA, ALU.mult), [pt + "qs", pt + "eA"], [rt + "qin"])
                yield
                A(lambda e: e.activation(eA, Pc, AF.Exp, scale=-1.0), [pt + "Pc", rt + "qin"], [pt + "eA"])
                yield
                V(lambda e: e.tensor_tensor(kin, kT, eA, ALU.mult), [pt + "kT", pt + "eA"], [rt + "kin"])
                yield
                V(lambda e: e.tensor_tensor(f3, totb, P3, ALU.subtract), [pt + "Pc"], [pt + "fT"])
                yield
            else:
                V(lambda e: e.tensor_tensor(fT, Pc, fT, ALU.subtract), [pt + "Pc", pt + "fT"], [pt + "fT"])
                yield
                V(lambda e: e.tensor_tensor(eA3, totb, f3, ALU.subtract), [pt + "Pc", pt + "fT"], [pt + "eA"])
                yield
                A(lambda e: e.activation(Pc, eA, AF.Exp), [pt + "eA", rt + "dec"], [pt + "Pc"])
                yield
                V(lambda e: e.tensor_tensor(qin, qs, Pc, ALU.mult), [pt + "qs", pt + "Pc"], [rt + "qin"])
                yield
                A(lambda e: e.activation(Pc, eA, AF.Exp, scale=-1.0), [pt + "eA", rt + "qin"], [pt + "Pc"])
                yield
                V(lambda e: e.tensor_tensor(kin, kT, Pc, ALU.mult), [pt + "kT", pt + "Pc"], [rt + "kin"])
                yield
            if k + 1 < len(blocks):
                projs(k + 1)
                yield
            A(lambda e: e.activation(fT, fT, AF.Exp), [pt + "fT"], [pt + "fT"])
            yield
            V(lambda e: e.tensor_tensor(kT, kT, fT, ALU.mult), [pt + "kT", pt + "fT", rt + "kin"], [pt + "kT"])
            yield
            A(lambda e: e.copy(eA, ps[k % 2][:, :]), ["ps.%d" % (k % 2), rt + "kin", rt + "qin"], [pt + "eA"])
            yield
        def stageA2(k):
            par = k % 2
            rt = "r%d" % par
            koTM, vTM = ctxs[k]["koTM"], ctxs[k]["vTM"]
            o3 = ps[5][:, :].rearrange("p (a c) -> p a c", c=128)
            petr([(o3[:, a, :], kT[:, a * 128:(a + 1) * 128], ident[:, :]) for a in range(4)], [pt + "kT", "ident"], ["ps.5"])
            A(lambda e: e.copy(koTM, o3), ["ps.5"], [rt + "koTM"])
            o3b = ps[2][:, :].rearrange("p (a c) -> p a c", c=128)
            petr([(o3b[:, a, :], eA[:, a * 128:(a + 1) * 128], ident[:, :]) for a in range(4)], [pt + "eA", "ident"], ["ps.2"])
            V(lambda e: e.tensor_copy(vTM, o3b), ["ps.2"], [rt + "vTM"])

        def stageB(k):
            hh, di, bi, tb = blocks[k]
            par = k % 2
            rt = "r%d" % par
            c = ctxs.pop(k)
            qin, kin, koTM, att, vTM, SentF, dec = c["qin"], c["kin"], c["koTM"], c["att"], c["vTM"], c["SentF"], c["dec"]
            tri = triF if di == 0 else triB
            tritok = "triF" if di == 0 else "triB"
            for sc in range(4):
                pe([(ps[5][:, sc * 128:(sc + 1) * 128], kin[:, sc * 128:(sc + 1) * 128], qin[:, sc * 128:(sc + 1) * 128], True, True)],
                   [rt + "kin", rt + "qin"], ["ps.5"])
                yield
            a3 = att.rearrange("p (a c) -> p a c", c=128)
            p53 = ps[5][:, :].rearrange("p (a c) -> p a c", c=128)
            trib = tri[:, None, :].broadcast_to([128, 4, 128])
            V(lambda e: e.tensor_tensor(a3, p53, trib, ALU.mult), ["ps.5", tritok], [rt + "att"])
            yield
            for n in range(8):
                a, r0 = n // 2, (n % 2) * 64
                pbk = 3 + n % 2
                pe([(ps[pbk][:, a * 128:(a + 1) * 128], koTM[r0:r0 + 64, a, :], vTM[r0:r0 + 64, a, :], True, True)],
                   [rt + "koTM", rt + "vTM"], ["ps.%d" % pbk])
                yield
            S = Sf[par].rearrange("p (i c) -> p i c", c=128)
            Sprev = Sf[1 - par].rearrange("p (i c) -> p i c", c=128)
            corder = list(range(8)) if di == 0 else list(range(7, -1, -1))
            if bi == 0:
                V(lambda e: e.memset(S[:, 0, :], 0.0), [], ["S%d_0" % par])
                yield
            else:
                V(lambda e: e.tensor_copy(S[:, 0, :], Sprev[:, 8, :]), ["S%d_8" % (1 - par)], ["S%d_0" % par])
                yield
            for i, n in enumerate(corder):
                pbk = 3 + n % 2
                dl = ps[pbk][:, (n // 2) * 128:(n // 2 + 1) * 128]
                V(lambda e, i=i, n=n, dl=dl: e.scalar_tensor_tensor(S[:, i + 1, :], S[:, i, :], dec[:, n:n + 1], dl, ALU.mult, ALU.add),
                  ["S%d_%d" % (par, i), rt + "dec", "ps.%d" % pbk], ["S%d_%d" % (par, i + 1)])
                yield
            A(lambda e: e.copy(SentF, S[:, 0:8, :]), ["S%d_%d" % (par, i) for i in range(8)], [rt + "SentF"])
            yield
            mms = []
            for sc in range(4):
                mms.append((ps[2][:, sc * 128:(sc + 1) * 128], vTM[:, sc, :], a3[:, sc, :], True, False))
                for cch in range(2):
                    n = sc * 2 + cch
                    i = corder.index(n)
                    mms.append((ps[2][:, n * 64:(n + 1) * 64], SentF[:, i, :], qin[:, n * 64:(n + 1) * 64], False, cch == 1))
            pe(mms, [rt + "vTM", rt + "att", rt + "SentF", rt + "qin"], ["ps.2"])
            yield
            dstc = catT[:, hh, tb * 512:(tb + 1) * 512]
            ctoks = tk("cat%d" % hh, tb, tb + 1)
            if di == 0:
                A(lambda e: e.copy(dstc, ps[2][:, :]), ["ps.2"], ctoks)
                yield
            else:
                V(lambda e: e.tensor_tensor(dstc, dstc.bitcast(F32), ps[2][:, :], ALU.add), ["ps.2"] + ctoks, ctoks)
                yield

        def epilogue(hh, par):
            rt = "r%d" % par
            wg_v, wg_t = wslice(w_in_d, 1024 + hh * 128)
            sgbs = [qs, fT, Pc, eA]
            stoks = [pt + "qs", pt + "fT", pt + "Pc", pt + "eA"]
            sqs = [rbuf(par, 2048), rbuf(par, 1536)]
            sqtk = [rt + "vTM", rt + "att"]
            t1 = kT
            gcol = cols[:, 16 + hh:17 + hh]
            for tb in range(4):
                pb = 2 if tb % 2 == 0 else 5
                projB(wg_v, wg_t, tb, pb)
                A(lambda e, tb=tb, pb=pb: e.activation(sgbs[tb], ps[pb][:, :], AF.Silu), ["ps.%d" % pb], [stoks[tb]])
            for tb in range(4):
                oc = catT[:, hh, tb * 512:(tb + 1) * 512]
                ctoks = tk("cat%d" % hh, tb, tb + 1)
                sq = sqs[tb % 2]
                pz = 3 + tb % 2
                A(lambda e, oc=oc, sq=sq: e.activation(sq, oc.bitcast(F32), AF.Square), ctoks, [sqtk[tb % 2]])
                pe([(ps[pz][:, :], onesR[:, :], sq, True, True)], [sqtk[tb % 2], "onesR"], ["ps.%d" % pz])
                V(lambda e, pz=pz: e.tensor_scalar(t1, ps[pz][:, :], 128.0 * RMS_EPS, None, ALU.add), ["ps.%d" % pz], [pt + "kT"])
                A(lambda e: e.activation(t1, t1, AF.Ln), [pt + "kT"], [pt + "kT"])
                A(lambda e: e.activation(t1, t1, AF.Exp, scale=-0.5), [pt + "kT"], [pt + "kT"])
                V(lambda e, tb=tb: e.scalar_tensor_tensor(t1, t1, gcol, sgbs[tb], ALU.mult, ALU.mult), [pt + "kT", stoks[tb], "c_hgn"], [pt + "kT"])
                V(lambda e, oc=oc: e.tensor_tensor(oc, oc.bitcast(F32), t1, ALU.mult), [pt + "kT"] + ctoks, ctoks)

        def run_interleaved(*gens):
            gens = [g for g in gens if g is not None]
            while gens:
                for g in list(gens):
                    try:
                        next(g)
                    except StopIteration:
                        gens.remove(g)

        projs(0)
        run_interleaved(stageA1(0))
        stageA2(0)
        for k in range(len(blocks)):
            run_interleaved(stageA1(k + 1) if k + 1 < len(blocks) else None, stageB(k))
            if k + 1 < len(blocks):
                stageA2(k + 1)
            hh, di, bi, tb = blocks[k]
            if di == 1 and bi == 3:
                epilogue(hh, k % 2)
        P.barrier()
        if stop == 1:
            P.add("pool", lambda e: e.nop(), [], ["DUMP"])
            dump_and_finish()
            P.emit(nc)
            return nc

        ring["base"], ring["n"] = 8704, 3
        wctr[0] = 0
        ropeC = Fb(0, 2048)
        ropeS = Fb(2048, 2048)
        tmpq = Fb(5120, 512)
        tmpvv = Fb(5632, 512)
        sqb = Rb(8192, 512)
        onesF = Fb(6144, 128)
        dma("sp", onesF, c_ones, w=["onesF"])
        dma("sp", ropeC, c_ropeC, w=["ropeC"])
        dma("sp", ropeS, c_ropeS, w=["ropeS"])
        qT = Rb(0, 2048)
        kTt = Rb(2048, 2048)
        VTM = Rb(4096, 2048).rearrange("p (a c) -> p a c", c=128)
        Er = [Rb(6144 + i * 512, 512) for i in range(4)]
        for hh in range(4):
            pstep = 0
            for which, dst, base, swb in (("q", qT, 2560, 0), ("k", kTt, 3072, 512)):
                w1, t1_ = wslice(w_in_d, base + hh * 128)
                w2, t2_ = wslice(w_sw_d, swb + hh * 128)
                for tb in range(4):
                    pa = 2 * (pstep % 2)
                    pb_ = pa + 1
                    tmpb = [tmpq, tmpvv][pstep % 2]
                    ttk = ["tmpq", "tmpvv"][pstep % 2]
                    pstep += 1
                    projB(w1, t1_, tb, pa)
                    projB(w2, t2_, tb, pb_)
                    d_ = dst[:, tb * 512:(tb + 1) * 512]
                    V(lambda e, tmpb=tmpb, tb=tb, pb_=pb_: e.tensor_tensor(tmpb, ps[pb_][:, :], ropeS[:, tb * 512:(tb + 1) * 512], ALU.mult),
                      ["ps.%d" % pb_, "ropeS"], [ttk])
                    V(lambda e, d_=d_, tb=tb, pa=pa: e.tensor_tensor(d_, ps[pa][:, :], ropeC[:, tb * 512:(tb + 1) * 512], ALU.mult),
                      ["ps.%d" % pa, "ropeC"], ["%sT.%d" % (which, tb)])
                    V(lambda e, d_=d_, tmpb=tmpb: e.tensor_tensor(d_, d_.bitcast(F32), tmpb, ALU.add),
                      [ttk, "%sT.%d" % (which, tb)], ["%sT.%d" % (which, tb)])
            wv_v, wv_t = wslice(w_in_d, 3584 + hh * 128)
            for tb in range(4):
                pv = 4 + tb % 2
                pt_ = 6 + tb % 2
                tmpb = [Fb(4096, 512), Fb(4608, 512)][tb % 2]
                ttk = ["r0", "r1"][tb % 2]
                projB(wv_v, wv_t, tb, pv)
                A(lambda e, tmpb=tmpb, pv=pv: e.copy(tmpb, ps[pv][:, :]), ["ps.%d" % pv], [ttk])
                o3 = ps[pt_][:, :].rearrange("p (a c) -> p a c", c=128)
                petr([(o3[:, a, :], tmpb[:, a * 128:(a + 1) * 128], ident[:, :]) for a in range(4)], [ttk, "ident"], ["ps.%d" % pt_])
                V(lambda e, tb=tb, o3=o3: e.tensor_copy(VTM[:, tb * 4:tb * 4 + 4, :], o3), ["ps.%d" % pt_], ["VTM.%d" % tb])
            tails = []
            for qb in range(4):
                steps = [(j, kp) for j in range(2) for kp in range(8)]
                stb = [(0, 1), (4, 5)]
                Zacc = [tmpq, tmpvv]
                ztok = ["tmpq", "tmpvv"]

                def st_mm(i):
                    j, kp = steps[i]
                    r0 = j * 64
                    bb = stb[i % 2]
                    pe([(ps[bb[h2]][:, :], kTt[r0:r0 + 64, (2 * kp + h2) * 128:(2 * kp + h2 + 1) * 128], qT[r0:r0 + 64, qb * 512:(qb + 1) * 512], True, True)
                        for h2 in range(2)], ["kT.%d" % (kp // 2), "qT.%d" % qb], ["ps.%d" % bb[0], "ps.%d" % bb[1]])

                st_mm(0)
                for i, (j, kp) in enumerate(steps):
                    if i + 1 < len(steps):
                        st_mm(i + 1)
                    bb = stb[i % 2]
                    ebs = [Er[(i % 2) * 2 + h2] for h2 in range(2)]
                    etk = ["E%d" % ((i % 2) * 2 + h2) for h2 in range(2)]
                    for h2 in range(2):
                        A(lambda e, Eb=ebs[h2], pb=bb[h2]: e.activation(Eb, ps[pb][:, :], AF.Exp, scale=0.125), ["ps.%d" % bb[h2]], [etk[h2]])
                    pe([(ps[2 + j][:, :], VTM[:, 2 * kp + h2, :], ebs[h2], (2 * kp + h2) == 0, (2 * kp + h2) == 15) for h2 in range(2)],
                       ["VTM.%d" % (kp // 2)] + etk, ["ps.%d" % (2 + j)])
                    for h2 in range(2):
                        if 2 * kp + h2 == 0:
                            V(lambda e, Z=Zacc[j], Eb=ebs[h2]: e.tensor_copy(Z, Eb.bitcast(F32)), [etk[h2]], [ztok[j]])
                        else:
                            V(lambda e, Z=Zacc[j], Eb=ebs[h2]: e.tensor_tensor(Z, Z, Eb.bitcast(F32), ALU.add), [etk[h2], ztok[j]], [ztok[j]])
                    if i in (1, 2) and tails:
                        tails.pop(0)()
                for j in range(2):
                    pe([(ps[6 + j][:, :], onesF, Zacc[j], True, True)], ["onesF", ztok[j]], ["ps.%d" % (6 + j)])
                r_0 = Fb(4096, 512)
                r_1 = Fb(4608, 512)
                A(lambda e, r_0=r_0: e.activation(r_0, ps[6][:, :], AF.Ln), ["ps.6"], ["r0"])
                A(lambda e, r_1=r_1: e.activation(r_1, ps[7][:, :], AF.Ln), ["ps.7"], ["r1"])
                A(lambda e, r_0=r_0: e.activation(r_0, r_0, AF.Exp, scale=-1.0), ["r0"], ["r0"])
                A(lambda e, r_1=r_1: e.activation(r_1, r_1, AF.Exp, scale=-1.0), ["r1"], ["r1"])
                V(lambda e, r_0=r_0: e.tensor_tensor(r_0, r_0, ps[2][:, :], ALU.mult), ["r0", "ps.2"], ["r0"])
                V(lambda e, r_1=r_1: e.tensor_tensor(r_1, r_1, ps[3][:, :], ALU.mult), ["r1", "ps.3"], ["r1"])
                def tail1(r_0=r_0, r_1=r_1):
                    V(lambda e: e.scalar_tensor_tensor(r_0, r_1, cols[:, 21:22], r_0, ALU.mult, ALU.add), ["r0", "r1", "c_nlam"], ["r0"])
                    A(lambda e: e.activation(sqb, r_0, AF.Square), ["r0"], ["sqb"])
                    pe([(ps[6][:, :], onesR[:, :], sqb, True, True)], ["sqb", "onesR"], ["ps.6"])

                def tail2(r_0=r_0, r_1=r_1, qb=qb, hh=hh):
                    V(lambda e: e.tensor_scalar(r_1, ps[6][:, :], 128.0 * RMS_EPS, None, ALU.add), ["ps.6", "r1"], ["r1"])
                    A(lambda e: e.activation(r_1, r_1, AF.Ln), ["r1"], ["r1"])
                    A(lambda e: e.activation(r_1, r_1, AF.Exp, scale=-0.5), ["r1"], ["r1"])
                    oc = catT[:, 4 + hh, qb * 512:(qb + 1) * 512]
                    V(lambda e: e.scalar_tensor_tensor(oc, r_0, cols[:, 20:21], r_1, ALU.mult, ALU.mult),
                      ["r0", "r1", "c_sub"], tk("cat%d" % (4 + hh), qb, qb + 1))

                tails += [tail1, tail2]
            while tails:
                tails.pop(0)()
        P.barrier()
        if stop == 2:
            P.add("pool", lambda e: e.nop(), [], ["DUMP"])
            dump_and_finish()
            P.emit(nc)
            return nc

        def load_wfull(w_d):
            v = SR[:, 0:8192].rearrange("p (j c) -> p j c", c=1024)
            for q4 in range(4):
                dma("pool", v[:, 2 * q4:2 * q4 + 2, :], w_d.rearrange("(j p) c -> p j c", p=128)[:, 2 * q4:2 * q4 + 2, :],
                    w=["WB.%d" % (2 * q4), "WB.%d" % (2 * q4 + 1)])
            return v

        def proj_ln(wfull, tm_dst=None):
            aI4 = SR[:, 8192:10240].rearrange("p (j c) -> p j c", c=512)
            dma("pool", SR[:, 8192:10240], c_aI4, w=["aI4"])

            def emit_mm(tc):
                p0 = 2 * (tc % 2)
                for b in range(2):
                    mms = []
                    for jj in range(4):
                        mms.append((ps[p0 + b][:, :], hT[:, 4 * b + jj, tc * 128:(tc + 1) * 128], aI4[:, jj, :], jj == 0, False))
                    for k in range(8):
                        mms.append((ps[p0 + b][:, :], catT[:, k, tc * 128:(tc + 1) * 128], wfull[:, k, b * 512:(b + 1) * 512], False, k == 7))
                    pe(mms, ["hT.%d" % tc, "aI4"] + ["cat%d.%d" % (k, tc // 4) for k in range(8)] + tk("WB", 0, 8), ["ps.%d" % (p0 + b)])

            emit_mm(0)
            for tc in range(16):
                p0 = 2 * (tc % 2)
                if tc + 1 < 16:
                    emit_mm(tc + 1)
                xb = xt[tc % 2]
                ln_chunk(ps[p0][:, :], ps[p0 + 1][:, :], ["ps.%d" % p0, "ps.%d" % (p0 + 1)], xb[:, :], ["xt%d" % (tc % 2)], xb[:, :], "xt%d" % (tc % 2), "ln")
                tm_to_fm(xb, ["xt%d" % (tc % 2)], tc, (4 + p0, 5 + p0))

        wfull = load_wfull(w_mix_d)
        load_ln(1)
        proj_ln(wfull)
        P.barrier()
        if stop == 3:
            P.add("pool", lambda e: e.nop(), [], ["DUMP"])
            dump_and_finish()
            P.emit(nc)
            return nc

        ring["base"], ring["n"] = 9216, 3
        wctr[0] = 0
        memTM = Fb(0, 2048).rearrange("p (a c) -> p a c", c=1024)
        memT = Rb(0, 2048).rearrange("p (j m) -> p j m", m=256)
        KmT = Rb(2048, 2048).rearrange("p (j m) -> p j m", m=256)
        Vm = Rb(4096, 2048).rearrange("p (a c) -> p a c", c=1024)
        qxT = Rb(6144, 2048).rearrange("p (a t) -> p a t", t=1024)
        Ex = [Rb(8192 + i * 512, 512) for i in range(2)]
        tmpv4 = Fb(4608, 512)
        for a in range(2):
            dma("sp", memTM[:, a, :], mem_d[a * 128:(a + 1) * 128, :], w=["memTM%d" % a])
        for a in range(2):
            for b in range(2):
                o3 = ps[6 + b][:, :].rearrange("p (q c) -> p q c", c=128)
                petr([(o3[:, q, :], memTM[:, a, (4 * b + q) * 128:(4 * b + q + 1) * 128], ident[:, :]) for q in range(4)],
                     ["memTM%d" % a, "ident"], ["ps.%d" % (6 + b)])
                copy_any(memT[:, 4 * b:4 * b + 4, a * 128:(a + 1) * 128], o3, ["ps.%d" % (6 + b)], ["memT"])
        for ec in range(8):
            wv_, wt_ = wslice(wk_d, ec * 128)
            pe([(ps[6][:, 0:256], wv_[:, j, :], memT[:, j, :], j == 0, j == 7) for j in range(8)], [wt_, "memT"], ["ps.6"])
            copy_any(KmT[:, ec, :], ps[6][:, 0:256], ["ps.6"], ["KmT"])
        for ec in range(8):
            wv_, wt_ = wslice(wv_d, ec * 128)
            pe([(ps[6][:, 0:256], wv_[:, j, :], memT[:, j, :], j == 0, j == 7) for j in range(8)], [wt_, "memT"], ["ps.6"])
            tmpv = tmpv4
            A(lambda e, tmpv=tmpv: e.copy(tmpv[:, 0:256], ps[6][:, 0:256]), ["ps.6"], ["tmpv4"])
            petr([(ps[7][:, a * 128:(a + 1) * 128], tmpv[:, a * 128:(a + 1) * 128], ident[:, :]) for a in range(2)], ["tmpv4", "ident"], ["ps.7"])
            V(lambda e, ec=ec: e.tensor_copy(Vm[:, :, ec * 128:(ec + 1) * 128], ps[7][:, 0:256].rearrange("p (a c) -> p a c", c=128)),
              ["ps.7"], ["Vm"])
        xoT = catT
        qx = [qxT, Rb(0, 2048).rearrange("p (a t) -> p a t", t=1024)]

        def qproj_gen(m):
            hx, half = divmod(m, 2)
            q = qx[m % 2]
            extra = ["memT"] if m % 2 == 1 else []
            for ec in range(2):
                wv_, wt_ = wslice(wq_d, hx * 256 + ec * 128)
                for tbl in range(2):
                    tb = half * 2 + tbl
                    pb = 6 + tbl
                    projB(wv_, wt_, tb, pb)
                    yield
                    copy_any(q[:, ec, tbl * 512:(tbl + 1) * 512], ps[pb][:, :], ["ps.%d" % pb], ["qx%d_%d.%d" % (m % 2, ec, tbl)] + extra)
                    yield

        def attn_gen(m):
            hx, half = divmod(m, 2)
            q = qx[m % 2]
            qk = "qx%d_" % (m % 2)
            for tbl in range(2):
                tb = half * 2 + tbl
                for mc in range(2):
                    pe([(ps[mc][:, :], KmT[:, hx * 2 + ec, mc * 128:(mc + 1) * 128], q[:, ec, tbl * 512:(tbl + 1) * 512], ec == 0, ec == 1)
                        for ec in range(2)], ["KmT", qk + "0.%d" % tbl, qk + "1.%d" % tbl], ["ps.%d" % mc])
                    yield
                    A(lambda e, mc=mc: e.activation(Ex[mc], ps[mc][:, :], AF.Exp, scale=1.0 / 16.0), ["ps.%d" % mc], ["Ex%d" % mc])
                    yield
                for ec2 in range(2):
                    pe([(ps[2 + ec2][:, :], Vm[:, mc, hx * 256 + ec2 * 128:hx * 256 + (ec2 + 1) * 128], Ex[mc], mc == 0, mc == 1)
                        for mc in range(2)], ["Vm", "Ex0", "Ex1"], ["ps.%d" % (2 + ec2)])
                    yield
                pe([(ps[4][:, :], onesR[:, :], Ex[mc], mc == 0, mc == 1) for mc in range(2)], ["onesR", "Ex0", "Ex1"], ["ps.4"])
                yield
                rz = Fb(4096, 512)
                A(lambda e, rz=rz: e.activation(rz, ps[4][:, :], AF.Ln), ["ps.4"], ["rz"])
                yield
                A(lambda e, rz=rz: e.activation(rz, rz, AF.Exp, scale=-1.0), ["rz"], ["rz"])
                yield
                for ec2 in range(2):
                    oc = xoT[:, hx * 2 + ec2, tb * 512:(tb + 1) * 512]
                    V(lambda e, oc=oc, rz=rz, ec2=ec2: e.tensor_tensor(oc, ps[2 + ec2][:, :], rz, ALU.mult),
                      ["ps.%d" % (2 + ec2), "rz"], tk("cat%d" % (hx * 2 + ec2), tb, tb + 1))
                    yield

        interleave(qproj_gen(0))
        for m in range(8):
            interleave(qproj_gen(m + 1) if m + 1 < 8 else None, attn_gen(m))
        P.barrier()
        wfull = load_wfull(wo_d)
        load_ln(2)
        proj_ln(wfull)
        P.barrier()
        if stop == 4:
            P.add("pool", lambda e: e.nop(), [], ["DUMP"])
            dump_and_finish()
            P.emit(nc)
            return nc

        h2TM = RB[:, :].rearrange("p (a c) -> p a c", c=1024)
        wr = Fb(4096, 128).rearrange("p (j c) -> p j c", c=16)
        dma("sp", wr, wr_d.rearrange("(j p) c -> p j c", p=128), w=["wr"])
        affT = FA[0:16, 0:2048]
        mskT = FA[0:16, 2048:4096]
        posT = FA[0:16, 4224:6272]
        mgT = affT
        ones16T = FA[0:16, 6272:6288]
        bs = FA[0:16, 6288:6304]
        iotaf = smallp[:, 0:256]
        posTM = smallp[:, 256:512]
        mgTM = smallp[:, 512:768]
        dma("sp", iotaf, c_iotaf, w=["iotaf"])
        V(lambda e: e.memset(ones16T, 1.0), [], ["ones16T"])
        for tb in range(4):
            pe([(ps[tb][0:16, :], wr[:, j, :], hT[:, j, tb * 512:(tb + 1) * 512].bitcast(F32), j == 0, j == 7) for j in range(8)],
               ["wr"] + tk("hT", tb * 4, tb * 4 + 4), ["ps.%d" % tb])
            A(lambda e, tb=tb: e.activation(affT[:, tb * 512:(tb + 1) * 512], ps[tb][0:16, :], AF.Exp), ["ps.%d" % tb], ["affT"])
        for tb in range(4):
            pe([(ps[4][0:16, :], ones16T[:, 0:16], affT[:, tb * 512:(tb + 1) * 512], True, True)], ["affT", "ones16T"], ["ps.4"])
            A(lambda e, tb=tb: e.activation(mskT[:, tb * 512:(tb + 1) * 512], ps[4][0:16, :], AF.Ln), ["ps.4"], ["mskT"])
            A(lambda e, tb=tb: e.activation(mskT[:, tb * 512:(tb + 1) * 512], mskT[:, tb * 512:(tb + 1) * 512], AF.Exp, scale=-1.0), ["mskT"], ["mskT"])
        V(lambda e: e.tensor_tensor(affT, affT, mskT, ALU.mult), ["affT", "mskT"], ["affT"])
        V(lambda e: e.memset(bs[:, 0:1], 0.0), [], ["bs"])
        for it in range(NITER):
            wi = 2.0 ** -(it + 1)
            V(lambda e, wi=wi: e.tensor_scalar(bs[:, 2:3], bs[:, 0:1], wi, None, ALU.add), ["bs"], ["bs"])
            V(lambda e: e.tensor_scalar(mskT, affT, bs[:, 2:3], 0.0, ALU.is_ge, ALU.add, accum_out=bs[:, 3:4]), ["bs", "affT"], ["mskT", "bs"])
            V(lambda e, wi=wi: e.tensor_scalar(bs[:, 4:5], bs[:, 3:4], float(CAP), wi, ALU.is_ge, ALU.mult), ["bs"], ["bs"])
            V(lambda e: e.tensor_tensor(bs[:, 0:1], bs[:, 0:1], bs[:, 4:5], ALU.add), ["bs"], ["bs"])
        V(lambda e: e.tensor_scalar(mskT, affT, bs[:, 0:1], None, ALU.is_ge), ["bs", "affT"], ["mskT"])
        V(lambda e: e.tensor_tensor_scan(posT, ones16T[:, 0:1].broadcast_to([16, T]), mskT, 0.0, ALU.mult, ALU.add), ["mskT", "ones16T"], ["posT"])
        V(lambda e: e.tensor_tensor(posT, posT, mskT, ALU.mult), ["posT", "mskT"], ["posT"])
        V(lambda e: e.tensor_scalar(posT, posT, -1.0, None, ALU.add), ["posT"], ["posT"])
        V(lambda e: e.tensor_tensor(mgT, mskT, affT, ALU.mult), ["mskT", "affT"], ["affT", "mgT"])
        for tc in range(16):
            for b in range(2):
                o3 = ps[6 + b][:, :].rearrange("p (q c) -> p q c", c=128)
                petr([(o3[:, q, :], hT[:, 4 * b + q, tc * 128:(tc + 1) * 128].bitcast(F32), ident[:, :]) for q in range(4)],
                     ["hT.%d" % tc, "ident"], ["ps.%d" % (6 + b)])
                A(lambda e, tc=tc, b=b: e.copy(h2TM[:, tc, b * 512:(b + 1) * 512], ps[6 + b][:, :]), ["ps.%d" % (6 + b)], ["h2TM.%d.%d" % (tc, b)])
        p3 = ps[5][:, 0:256].rearrange("p (a c) -> p a c", c=16)
        petr([(p3[:, tc, :], posT[:, tc * 128:(tc + 1) * 128], ident[0:16, 0:16]) for tc in range(16)], ["posT", "ident"], ["ps.5"])
        V(lambda e: e.tensor_copy(posTM, ps[5][:, 0:256]), ["ps.5"], ["posTM"])
        p3b = ps[4][:, 0:256].rearrange("p (a c) -> p a c", c=16)
        petr([(p3b[:, tc, :], mgT[:, tc * 128:(tc + 1) * 128], ident[0:16, 0:16]) for tc in range(16)], ["mgT", "ident"], ["ps.4"])
        V(lambda e: e.tensor_copy(mgTM, ps[4][:, 0:256]), ["ps.4"], ["mgTM"])
        acc_w = RA[:, :].rearrange("p (a c) -> p a c", c=1024)
        acc = RA[:, :].bitcast(F32).rearrange("p (a c) -> p a c", c=1024)
        for tc in range(16):
            A(lambda e, tc=tc: e.mul(acc_w[:, tc, :], h2TM[:, tc, :].bitcast(F32), ALPHA),
              ["h2TM.%d.0" % tc, "h2TM.%d.1" % tc] + tk("hT", 0, 16), ["acc.%d.0" % tc, "acc.%d.1" % tc])
        P.barrier()
        if stop == 5:
            P.add("pool", lambda e: e.nop(), [], ["DUMP"])
            dump_and_finish()
            P.emit(nc)
            return nc

        cd3 = Rb(0, 2048).rearrange("p (a c) -> p a c", c=1024)
        xs3 = Rb(2048, 2048).rearrange("p (j c) -> p j c", c=256)
        selp = [Rb(4096 + i * 256, 256) for i in range(2)]
        actT = [Rb(4608 + i * 256, 256) for i in range(2)]
        SelGh = [Rb(5120 + i * 512, 512).rearrange("p (a t) -> p a t", t=256) for i in range(2)]
        xst3 = Fb(0, 2048).rearrange("p (a c) -> p a c", c=1024)
        sgt2 = [Fb(4096, 256), Fb(4864, 256)]
        sgtm = [Fb(4352 + i * 256, 256) for i in range(2)]

        def scatter_items(ex):
            items = []
            for hb in range(8):
                SG = SelGh[hb % 2]
                sgk = "SelG%d" % (hb % 2)

                def prep(hb=hb, SG=SG, sgk=sgk):
                    for t in range(2):
                        tc = hb * 2 + t
                        sm = sgtm[t]
                        V(lambda e, sm=sm, tc=tc: e.tensor_scalar(sm, iotaf, posTM[:, tc * 16 + ex:tc * 16 + ex + 1], mgTM[:, tc * 16 + ex:tc * 16 + ex + 1], ALU.is_equal, ALU.mult),
                          ["iotaf", "posTM", "mgTM"], ["sgtm%d" % t])
                        petr([(ps[6][:, (cc * 2 + t) * 128:(cc * 2 + t + 1) * 128], sm[:, cc * 128:(cc + 1) * 128], ident[:, :]) for cc in range(2)],
                             ["sgtm%d" % t, "ident"], ["ps.6"])
                    copy_any(SG, ps[6][:, :].rearrange("p (a t) -> p a t", t=256), ["ps.6"], [sgk])
                items.append(prep)
                for t in range(2):
                    for db in range(2):
                        def tile(hb=hb, t=t, db=db, SG=SG, sgk=sgk):
                            tc = hb * 2 + t
                            pe([(ps[7][:, :], SG[:, cc, t * 128:(t + 1) * 128], cd3[:, cc, db * 512:(db + 1) * 512], cc == 0, cc == 1)
                                for cc in range(2)], [sgk, "cd.0.%d" % db, "cd.1.%d" % db], ["ps.7"])
                            V(lambda e: e.tensor_tensor(acc_w[:, tc, db * 512:(db + 1) * 512], acc[:, tc, db * 512:(db + 1) * 512], ps[7][:, :], ALU.add),
                              ["ps.7", "acc.%d.%d" % (tc, db)], ["acc.%d.%d" % (tc, db)])
                        items.append(tile)
            return items

        pending = []

        def drain(n):
            for _ in range(n):
                if pending:
                    pending.pop(0)()

        for ex in range(16):
            for tc in range(16):
                sp_ = selp[tc % 2]
                V(lambda e, sp_=sp_, tc=tc, ex=ex: e.tensor_scalar(sp_, iotaf, posTM[:, tc * 16 + ex:tc * 16 + ex + 1], None, ALU.is_equal),
                  ["iotaf", "posTM"], ["selp%d" % (tc % 2)])
                pe([(ps[dch // 2][:, (dch % 2) * 256:(dch % 2 + 1) * 256], h2TM[:, tc, dch * 128:(dch + 1) * 128], sp_,
                     tc == 0 and dch % 2 == 0, tc == 15) for dch in range(8)],
                   ["selp%d" % (tc % 2), "h2TM.%d.0" % tc, "h2TM.%d.1" % tc], tk("ps", 0, 4), skip=True)
            for b4 in range(4):
                copy_any(xs3[:, 2 * b4:2 * b4 + 2, :], ps[b4][:, :].rearrange("p (a c) -> p a c", c=256), ["ps.%d" % b4], ["xsT.%d" % b4])

            def dma_down(fb, ex=ex):
                sset = fb % 2
                dma("pool", SR[:, 10240 + sset * 1024:10240 + (sset + 1) * 1024], wd_d[ex][fb * 128:(fb + 1) * 128, :], w=["WS%dd" % sset])

            def ffn_front(fb, ex=ex):
                sset = fb % 2
                o = 6144 + sset * 2048
                wgs = SR[:, o:o + 1024].rearrange("p (j c) -> p j c", c=128)
                wus = SR[:, o + 1024:o + 2048].rearrange("p (j c) -> p j c", c=128)
                wds = SR[:, 10240 + sset * 1024:10240 + (sset + 1) * 1024]
                wt = "WS%d" % sset
                dma("pool", SR[:, o:o + 1024], wg_d[ex, fb], w=[wt + "g"])
                dma("pool", SR[:, o + 1024:o + 2048], wu_d[ex, fb], w=[wt + "u"])
                if fb >= 1:
                    dma_down(fb - 1)
                pe([(ps[4][:, 0:256], wgs[:, j, :], xs3[:, j, :], j == 0, j == 7) for j in range(8)], [wt + "g"] + ["xsT.%d" % b_ for b_ in range(4)], ["ps.4"])
                drain(1)
                pe([(ps[5][:, 0:256], wus[:, j, :], xs3[:, j, :], j == 0, j == 7) for j in range(8)], [wt + "u"] + ["xsT.%d" % b_ for b_ in range(4)], ["ps.5"])
                sg_ = sgt2[sset]
                at_ = actT[sset]
                A(lambda e, sg_=sg_: e.activation(sg_, ps[4][:, 0:256], AF.Silu), ["ps.4"], ["sgt%d" % sset])
                V(lambda e, at_=at_, sg_=sg_: e.tensor_tensor(at_, sg_, ps[5][:, 0:256], ALU.mult),
                  ["sgt%d" % sset, "ps.5"], ["actT%d" % sset])
                drain(1)

            def ffn_down(fb):
                sset = fb % 2
                wds = SR[:, 10240 + sset * 1024:10240 + (sset + 1) * 1024]
                at_ = actT[sset]
                pe([(ps[cc * 2 + db][:, :], at_[:, cc * 128:(cc + 1) * 128], wds[:, db * 512:(db + 1) * 512], fb == 0, fb == 15)
                    for cc in range(2) for db in range(2)], ["actT%d" % sset, "WS%dd" % sset], tk("ps", 0, 4))
                drain(1)

            ffn_front(0)
            for fb in range(16):
                if fb + 1 < 16:
                    ffn_front(fb + 1)
                else:
                    dma_down(15)
                ffn_down(fb)
            drain(len(pending))
            for cc in range(2):
                for db in range(2):
                    copy_any(cd3[:, cc, db * 512:(db + 1) * 512], ps[cc * 2 + db][:, :], ["ps.%d" % (cc * 2 + db)], ["cd.%d.%d" % (cc, db)])
            pending = scatter_items(ex)
        drain(len(pending))
        load_ln(3)
        def ln3_chain(tc):
            par = tc % 2
            xb = xt[par]
            xk = "xt%d" % par
            yield from ln_chunk_gen(acc[:, tc, 0:512], acc[:, tc, 512:1024], ["acc.%d.0" % tc, "acc.%d.1" % tc], xb[:, :], [xk], xb[:, :], xk, par)
            dma("sp", out_d[tc * 128:(tc + 1) * 128, :], xb[:, :], r=[xk], outp=True)
            yield

        for tc in range(0, 16, 2):
            interleave(ln3_chain(tc), ln3_chain(tc + 1))
        P.emit(nc)
    return nc


def _consts():
    c = {}
    c["c_ident"] = np.eye(128, dtype=np.float32)
    c["c_aI"] = (np.eye(128) * ALPHA).astype(np.float32)
    a4 = np.zeros((128, 4, 512), np.float32)
    for j in range(4):
        a4[np.arange(128), j, j * 128 + np.arange(128)] = ALPHA
    c["c_aI4"] = np.ascontiguousarray(a4.reshape(128, 2048))
    c["c_ones"] = np.ones((128, 128), np.float32)
    s = np.arange(128)[:, None]
    cc = np.arange(128)[None, :]
    same = (s // 64) == (cc // 64)
    c["c_triF"] = (same & (s <= cc)).astype(np.float32)
    c["c_triB"] = (same & (s >= cc)).astype(np.float32)
    rm = np.ones((128, 512), np.float32)
    rm[:, ::64] = 0.0
    c["c_rmask"] = rm
    inv = 1.0 / (500000.0 ** (np.arange(0, 16, 2, dtype=np.float32) / 16.0))
    ang = np.arange(T, dtype=np.float32)[None, :] * inv[:, None]
    C = np.ones((128, T), np.float32)
    S = np.zeros((128, T), np.float32)
    for comp in range(2):
        b = comp * 64
        C[b:b + 8] = np.cos(ang)
        C[b + 8:b + 16] = np.cos(ang)
        S[b:b + 8] = -np.sin(ang)
        S[b + 8:b + 16] = np.sin(ang)
    c["c_ropeC"] = C
    c["c_ropeS"] = S
    c["c_iotaf"] = np.tile(np.arange(256, dtype=np.float32)[None, :], (128, 1))
    return c


def _swap_cols(w_in):
    out = np.empty((D, 1024), np.float32)
    for bi, base in enumerate((2560, 3072)):
        blk = w_in[:, base:base + 512].reshape(D, 8, 64)
        sw = blk.copy()
        sw[:, :, 0:8] = blk[:, :, 8:16]
        sw[:, :, 8:16] = blk[:, :, 0:8]
        out[:, bi * 512:(bi + 1) * 512] = sw.reshape(D, 512)
    return out


def _ffn_layout(w):
    e = w.shape[0]
    v = w.reshape(e, 8, 128, 16, 128).transpose(0, 3, 2, 1, 4)
    return np.ascontiguousarray(v).reshape(e, 16, 128, 1024)


_NC_CACHE = {}


def _prep_inputs(inp):
    f = lambda a: np.ascontiguousarray(np.asarray(a, dtype=np.float32))
    w_in = f(inp["w_in"])[0]
    shared = {
        "w_in": w_in,
        "w_sw": _swap_cols(w_in),
        "w_mix": f(inp["w_mix_out"])[0],
        "xa_wq": f(inp["xa_wq"])[0], "xa_wk": f(inp["xa_wk"])[0], "xa_wv": f(inp["xa_wv"])[0], "xa_wo": f(inp["xa_wo"])[0],
        "w_router": f(inp["w_router"])[0],
        "w_gate": _ffn_layout(f(inp["w_gate"])[0]), "w_up": _ffn_layout(f(inp["w_up"])[0]), "w_down": f(inp["w_down"])[0],
        "ln_g": np.stack([f(inp["emb_ln_g"]), f(inp["ln1_g"])[0], f(inp["ln2_g"])[0], f(inp["ln3_g"])[0]]),
        "ln_b": np.stack([f(inp["emb_ln_b"]), f(inp["ln1_b"])[0], f(inp["ln2_b"])[0], f(inp["ln3_b"])[0]]),
        "hg_lb": f(inp["hg_lb_logits"]).reshape(16, 128),
        "hg_norm_g": f(inp["hg_norm_g"]).reshape(4, 128),
        "da_subln_g": f(inp["da_subln_g"]).reshape(1, 128),
        "da_lam": np.stack([f(inp["da_lambda_q1"])[0], f(inp["da_lambda_k1"])[0], f(inp["da_lambda_q2"])[0], f(inp["da_lambda_k2"])[0]]),
    }
    shared.update(_consts())
    x = f(inp["x"])
    mem = f(inp["mem"])
    return [dict(shared, x=x[b], mem=mem[b]) for b in range(8)]


def kernel(**inputs):
    if "full" not in _NC_CACHE:
        _NC_CACHE["full"] = build(None)
    nc = _NC_CACHE["full"]
    in_maps = _prep_inputs(inputs)
    res = run_bass_kernel_spmd(nc, in_maps, core_ids=list(range(8)))
    return np.stack([np.asarray(r["out"], dtype=np.float32) for r in res.results], axis=0)
```

```python
import contextlib
import math
import numpy as np
import concourse.bass as bass
import concourse.mybir as mybir
from concourse.bass_utils import run_bass_kernel_spmd

F32 = mybir.dt.float32
F32R = mybir.dt.float32r
AF = mybir.ActivationFunctionType
ALU = mybir.AluOpType
AX = mybir.AxisListType

T = 2048
D = 1024
ALPHA = 2.0 ** 0.25
LN_EPS = 1e-5
RMS_EPS = 1e-6
LAMBDA_INIT = 0.8 - 0.6 * math.exp(0.0)
CAP = 256
NITER = 28
import os
_DBG_SKIP64 = os.environ.get('DBG_SKIP64') == '1'
ENGS = ["pe", "act", "dve", "pool", "sp"]


class Op:
    __slots__ = ("i", "eng", "fn", "deps", "dma", "out")

    def __init__(self, i, eng, fn, deps, dma, out):
        self.i, self.eng, self.fn, self.deps, self.dma, self.out = i, eng, fn, deps, dma, out


class Prog:
    def __init__(self):
        self.ops = []
        self.lastw = {}
        self.readers = {}
        self.since_barrier = []
        self.barrier_op = None

    def add(self, eng, fn, r=(), w=(), dma=False, out=False):
        i = len(self.ops)
        deps = set()
        if self.barrier_op is not None:
            deps.add(self.barrier_op)
        for t in r:
            if t in self.lastw:
                deps.add(self.lastw[t])
        for t in w:
            if t in self.lastw:
                deps.add(self.lastw[t])
            deps.update(self.readers.get(t, ()))
        for t in r:
            self.readers.setdefault(t, []).append(i)
        for t in w:
            self.lastw[t] = i
            self.readers[t] = []
        deps.discard(i)
        self.ops.append(Op(i, eng, fn, deps, dma, out))
        self.since_barrier.append(i)
        return i

    def barrier(self):
        i = len(self.ops)
        deps = set(self.since_barrier)
        self.ops.append(Op(i, "pool", lambda e: e.nop(), deps, False, False))
        self.since_barrier = []
        self.barrier_op = i

    def emit(self, nc, n_dma_sems=10):
        ops = self.ops
        n = len(ops)
        needs = [False] * n
        for op in ops:
            for d in op.deps:
                needs[d] = True
        with contextlib.ExitStack() as st:
            csem = {e: st.enter_context(nc.semaphore("c_" + e)) for e in ENGS}
            dsems = {e: [st.enter_context(nc.semaphore("d_%s%d" % (e, k))) for k in range(n_dma_sems)]
                     for e in ("sp", "act", "pool")}
            duse = {e: [0] * n_dma_sems for e in dsems}
            drr = {e: 0 for e in dsems}
            cnt = {e: 0 for e in ENGS}
            sig = [None] * n
            vc = [None] * n
            engvc = {e: {} for e in ENGS}
            dknown = {e: set() for e in ENGS}
            plan = {e: [] for e in ENGS}
            out_dmas = []
            for op in ops:
                e = op.eng
                my = engvc[e]
                waits = []
                for d in sorted(op.deps):
                    dop = ops[d]
                    if dop.dma:
                        if d in dknown[e]:
                            continue
                        dknown[e].add(d)
                        waits.append(sig[d])
                    else:
                        if dop.eng == "pe" and e == "pe":
                            continue
                        c = sig[d][1]
                        if my.get(dop.eng, 0) >= c:
                            continue
                        waits.append((csem[dop.eng], c))
                    for k, v in vc[d].items():
                        if my.get(k, 0) < v:
                            my[k] = v
                if op.dma:
                    k = drr[e]
                    drr[e] = (k + 1) % n_dma_sems
                    s = dsems[e][k]
                    if duse[e][k] > 0:
                        waits.append((s, 16 * duse[e][k]))
                    duse[e][k] += 1
                    sig[op.i] = (s, 16 * duse[e][k])
                    vc[op.i] = dict(my)
                    plan[e].append((waits, op, (s, 16)))
                    if op.out:
                        out_dmas.append(sig[op.i])
                else:
                    if needs[op.i]:
                        cnt[e] += 1
                        sig[op.i] = (csem[e], cnt[e])
                        v = dict(my)
                        v[e] = cnt[e]
                        vc[op.i] = v
                        plan[e].append((waits, op, (csem[e], 1)))
                    else:
                        plan[e].append((waits, op, None))
            self.stats = {e: len(plan[e]) for e in ENGS}

            def run(eng, e):
                for waits, op, s in plan[e]:
                    for sem, val in waits:
                        eng.wait_ge(sem, val)
                    ins = op.fn(eng)
                    if s is not None:
                        ins.then_inc(s[0], s[1])
                if e == "sp":
                    for sem, val in out_dmas:
                        eng.wait_ge(sem, val)

            with nc.Block() as block:
                @block.tensor
                def _(eng):
                    run(eng, "pe")

                @block.scalar
                def _(eng):
                    run(eng, "act")

                @block.vector
                def _(eng):
                    run(eng, "dve")

                @block.gpsimd
                def _(eng):
                    run(eng, "pool")

                @block.sync
                def _(eng):
                    run(eng, "sp")


def tk(name, lo, hi):
    return ["%s.%d" % (name, i) for i in range(lo, hi)]


def build(stop=None):
    nc = bass.Bass("TRN2", target_bir_lowering=False)
    P = Prog()

    def din(name, shape):
        return nc.dram_tensor(name, list(shape), F32, kind="ExternalInput").ap()

    x_d = din("x", [T, D])
    mem_d = din("mem", [256, D])
    w_in_d = din("w_in", [D, 4096])
    w_sw_d = din("w_sw", [D, 1024])
    w_mix_d = din("w_mix", [D, D])
    wq_d = din("xa_wq", [D, D])
    wk_d = din("xa_wk", [D, D])
    wv_d = din("xa_wv", [D, D])
    wo_d = din("xa_wo", [D, D])
    wr_d = din("w_router", [D, 16])
    if stop is None or stop >= 6:
        wg_d = din("w_gate", [16, 16, 128, 1024])
        wu_d = din("w_up", [16, 16, 128, 1024])
        wd_d = din("w_down", [16, 2048, D])
    lng_d = din("ln_g", [4, D])
    lnb_d = din("ln_b", [4, D])
    lb_d = din("hg_lb", [16, 128])
    hgn_d = din("hg_norm_g", [4, 128])
    sub_d = din("da_subln_g", [1, 128])
    lam_d = din("da_lam", [4, 64])
    c_ident = din("c_ident", [128, 128])
    c_aI = din("c_aI", [128, 128])
    c_aI4 = din("c_aI4", [128, 2048])
    c_ones = din("c_ones", [128, 128])
    c_triF = din("c_triF", [128, 128])
    c_triB = din("c_triB", [128, 128])
    c_rmask = din("c_rmask", [128, 512])
    c_ropeC = din("c_ropeC", [128, T])
    c_ropeS = din("c_ropeS", [128, T])
    c_iotaf = din("c_iotaf", [128, 256])
    out_d = nc.dram_tensor("out", [T, D], F32, kind="ExternalOutput").ap()
    dbg = stop is not None
    if dbg:
        dbgA_d = nc.dram_tensor("dbgA", [128, 16384], F32, kind="ExternalOutput").ap()
        dbgB_d = nc.dram_tensor("dbgB", [128, 16384], F32, kind="ExternalOutput").ap()

    st = contextlib.ExitStack()
    with st:
        def sb(name, shape, dt=F32):
            return st.enter_context(nc.sbuf_tensor(name, list(shape), dt))

        RA = sb("RA", [128, 16384], F32R)
        RB = sb("RB", [128, 16384], F32R)
        SR = sb("SR", [128, 12288], F32R)
        FA = sb("FA", [128, 6400], F32)

        def Rb(off, n):
            return SR[:, off:off + n]

        def Fb(off, n):
            return FA[:, off:off + n]
        smallp = sb("smallp", [128, 768], F32)
        ident = sb("ident", [128, 128])
        aI = sb("aI", [128, 128])
        onesR = sb("onesR", [128, 128], F32R)
        gB = FA[:, 2048:3072]
        bB = FA[:, 3072:4096]
        xt = [FA[:, i * 1024:(i + 1) * 1024] for i in range(2)]
        stt = sb("stt", [128, 32])
        cols = sb("cols", [128, 64])
        ps = [st.enter_context(nc.psum_tensor("ps%d" % i, [128, 512], F32)) for i in range(8)]

        hT = RA[:, :].rearrange("p (j t) -> p j t", t=T)
        catT = RB[:, :].rearrange("p (j t) -> p j t", t=T)

        def dma(q, out, in_, r=(), w=(), outp=False):
            return P.add(q, lambda e: e.dma_start(out=out, in_=in_), r, w, dma=True, out=outp)

        def pe(mms, r, w, skip=False):
            def f(e):
                for (o, l, rh, s, t) in mms:
                    ins = e.matmul(o, l, rh, start=s, stop=t, skip_group_check=skip)
                return ins
            return P.add("pe", f, r, w)

        def petr(trs, r, w):
            def f(e):
                for (o, i_, idn) in trs:
                    ins = e.transpose(o, i_, idn)
                return ins
            return P.add("pe", f, r, w)

        def A(fn, r, w):
            return P.add("act", fn, r, w)

        def V(fn, r, w):
            return P.add("dve", fn, r, w)

        _alt = [0]

        def copy_any(out, in_, r, w):
            _alt[0] ^= 1
            if _alt[0]:
                return A(lambda e: e.copy(out, in_), r, w)
            return V(lambda e: e.tensor_copy(out, in_), r, w)

        if dbg:
            V(lambda e: e.memset(RA[:, :], 0.0), [], tk("hT", 0, 16))
            V(lambda e: e.memset(RB[:, :], 0.0), [], ["cat%d.%d" % (k, q) for k in range(8) for q in range(4)])
        dma("sp", ident[:, :], c_ident, w=["ident"])
        dma("sp", aI[:, :], c_aI, w=["aI"])
        dma("pool", onesR[:, :], c_ones, w=["onesR"])

        def load_ln(k):
            dma("sp", gB, lng_d[k].partition_broadcast(128), w=["gB"])
            dma("sp", bB, lnb_d[k].partition_broadcast(128), w=["bB"])

        def ln_chunk_gen(src_lo, src_hi, rtok, dst, dtok, tmp, ttok, par=0):
            so = 16 * par
            sx = "%d" % par
            s6 = stt[:, so:so + 12].rearrange("p (a b) -> p a b", b=6)
            mean, var, rstd, nmr = (stt[:, so + 12:so + 13], stt[:, so + 13:so + 14], stt[:, so + 14:so + 15], stt[:, so + 15:so + 16])
            V(lambda e: e.bn_stats(s6[:, 0, :], src_lo), rtok, ["st6a" + sx])
            yield
            V(lambda e: e.bn_stats(s6[:, 1, :], src_hi), rtok, ["st6b" + sx])
            yield
            V(lambda e: e.bn_aggr(stt[:, so + 12:so + 14], stt[:, so:so + 12]), ["st6a" + sx, "st6b" + sx], ["mv" + sx])
            yield
            V(lambda e: e.tensor_scalar(rstd, var, LN_EPS, None, ALU.add), ["mv" + sx], ["rstd" + sx])
            yield
            A(lambda e: e.activation(rstd, rstd, AF.Ln), ["rstd" + sx], ["rstd" + sx])
            yield
            A(lambda e: e.activation(rstd, rstd, AF.Exp, scale=-0.5), ["rstd" + sx], ["rstd" + sx])
            yield
            V(lambda e: e.scalar_tensor_tensor(nmr, mean, -1.0, rstd, ALU.mult, ALU.mult), ["mv" + sx, "rstd" + sx], ["nmr" + sx])
            yield
            A(lambda e: e.activation(tmp[:, 0:512], src_lo, AF.Identity, bias=nmr, scale=rstd),
              rtok + ["rstd" + sx, "nmr" + sx], [ttok + "a"] + list(dtok))
            yield
            A(lambda e: e.activation(tmp[:, 512:1024], src_hi, AF.Identity, bias=nmr, scale=rstd),
              rtok + ["rstd" + sx, "nmr" + sx], [ttok + "b"] + list(dtok))
            yield
            V(lambda e: e.tensor_tensor(tmp, tmp, gB, ALU.mult), [ttok + "a", ttok + "b", "gB"], [ttok + "a", ttok + "b"])
            yield
            V(lambda e: e.tensor_tensor(dst, tmp, bB, ALU.add), [ttok + "a", ttok + "b", "bB"], dtok)
            yield

        def ln_chunk(src_lo, src_hi, rtok, dst, dtok, tmp, ttok, key):
            for _ in ln_chunk_gen(src_lo, src_hi, rtok, dst, dtok, tmp, ttok, 0):
                pass

        def interleave(*gens):
            gens = [g for g in gens if g is not None]
            while gens:
                for g in list(gens):
                    try:
                        next(g)
                    except StopIteration:
                        gens.remove(g)

        def tm_to_fm(src, stok, tc, pbanks):
            for b in range(2):
                pb = pbanks[b]
                o3 = ps[pb][:, :].rearrange("p (a c) -> p a c", c=128)
                petr([(o3[:, a, :], src[:, (4 * b + a) * 128:(4 * b + a + 1) * 128], ident[:, :]) for a in range(4)],
                     stok + ["ident"], ["ps.%d" % pb])
                copy_any(hT[:, 4 * b:4 * b + 4, tc * 128:(tc + 1) * 128], o3, ["ps.%d" % pb], ["hT.%d" % tc])

        def dump_and_finish():
            dma("sp", dbgA_d, RA[:, :].bitcast(F32), r=["DUMP"], outp=True)
            dma("sp", dbgB_d, RB[:, :].bitcast(F32), r=["DUMP"], outp=True)

        load_ln(0)
        def ln0_chain(tc):
            par = tc % 2
            xb = xt[par]
            xk = "xt%d" % par
            dma("sp", xb[:, :], x_d[tc * 128:(tc + 1) * 128, :], w=[xk])
            yield
            yield from ln_chunk_gen(xb[:, 0:512], xb[:, 512:1024], [xk], xb[:, :], [xk], xb[:, :], xk, par)
            tm_to_fm(xb, [xk], tc, (6, 7) if par == 0 else (4, 5))
            yield

        for tc in range(0, 16, 2):
            interleave(ln0_chain(tc), ln0_chain(tc + 1))

        lbt = FA[0:16, 4096:4224]
        dma("sp", lbt, lb_d, w=["lbt"])
        petr([(ps[0][:, 0:16], lbt, ident[0:16, 0:16])], ["lbt", "ident"], ["ps.0"])
        V(lambda e: e.tensor_copy(cols[:, 48:64], ps[0][:, 0:16]), ["ps.0"], ["c48"])
        psl = cols[:, 48:64].rearrange("p (a l h) -> p a l h", a=2, l=2)
        c_lb = cols[:, 0:8].rearrange("p (a h) -> p a h", a=2)
        c_oml = cols[:, 8:16].rearrange("p (a h) -> p a h", a=2)
        V(lambda e: e.tensor_tensor(c_lb, psl[:, :, 1, :], psl[:, :, 0, :], ALU.subtract), ["c48"], ["c_lb"])
        A(lambda e: e.activation(cols[:, 0:8], cols[:, 0:8], AF.Exp), ["c_lb"], ["c_lb"])
        V(lambda e: e.tensor_scalar(cols[:, 0:8], cols[:, 0:8], 1.0, None, ALU.add), ["c_lb"], ["c_lb"])
        V(lambda e: e.reciprocal(cols[:, 0:8], cols[:, 0:8]), ["c_lb"], ["c_lb"])
        V(lambda e: e.tensor_scalar(cols[:, 8:16], cols[:, 0:8], -1.0, 1.0, ALU.mult, ALU.add), ["c_lb"], ["c_oml"])
        V(lambda e: e.tensor_scalar(cols[:, 40:48], cols[:, 8:16], 0.5, None, ALU.mult), ["c_oml"], ["c_oml"])
        V(lambda e: e.tensor_tensor(cols[:, 24:32], cols[:, 0:8], cols[:, 40:48], ALU.add), ["c_lb", "c_oml"], ["c_lb"])
        gt = FA[0:4, 4224:4352]
        dma("sp", gt, hgn_d, w=["gt"])
        petr([(ps[1][:, 0:4], gt, ident[0:4, 0:4])], ["gt", "ident"], ["ps.1"])
        V(lambda e: e.tensor_scalar(cols[:, 16:20], ps[1][:, 0:4], math.sqrt(128.0), None, ALU.mult), ["ps.1"], ["c_hgn"])
        sg1 = FA[0:1, 4352:4480]
        dma("sp", sg1, sub_d, w=["sg1"])
        petr([(ps[2][:, 0:1], sg1, ident[0:1, 0:1])], ["sg1", "ident"], ["ps.2"])
        V(lambda e: e.tensor_scalar(cols[:, 20:21], ps[2][:, 0:1], math.sqrt(128.0) * (1.0 - LAMBDA_INIT), None, ALU.mult),
          ["ps.2"], ["c_sub"])
        lmb = FA[:, 4480:4736].rearrange("p (a b) -> p a b", b=64)
        for a in range(4):
            dma("sp", lmb[:, a, :], lam_d[a].partition_broadcast(128), w=["lmb%d" % a])
        V(lambda e: e.tensor_tensor(lmb[:, 0, :], lmb[:, 0, :], lmb[:, 1, :], ALU.mult), ["lmb0", "lmb1"], ["lmb0"])
        V(lambda e: e.tensor_tensor(lmb[:, 2, :], lmb[:, 2, :], lmb[:, 3, :], ALU.mult), ["lmb2", "lmb3"], ["lmb2"])
        V(lambda e: e.reduce_sum(cols[:, 22:23], lmb[:, 0, :], axis=AX.X), ["lmb0"], ["c22"])
        V(lambda e: e.reduce_sum(cols[:, 23:24], lmb[:, 2, :], axis=AX.X), ["lmb2"], ["c23"])
        A(lambda e: e.activation(cols[:, 22:24], cols[:, 22:24], AF.Exp), ["c22", "c23"], ["c22", "c23"])
        V(lambda e: e.tensor_tensor(cols[:, 21:22], cols[:, 23:24], cols[:, 22:23], ALU.subtract), ["c22", "c23"], ["c_nlam"])
        V(lambda e: e.tensor_scalar(cols[:, 21:22], cols[:, 21:22], -LAMBDA_INIT, None, ALU.add), ["c_nlam"], ["c_nlam"])
        P.barrier()

        if stop == 0:
            P.add("pool", lambda e: e.nop(), [], ["DUMP"])
            dump_and_finish()
            P.emit(nc)
            return nc

        wctr = [0]
        ring = {"base": 3584, "n": 7}

        def wslice(w_d, c0):
            k = wctr[0] % ring["n"]
            wctr[0] += 1
            o_ = ring["base"] + k * 1024
            v = SR[:, o_:o_ + 1024].rearrange("p (j c) -> p j c", c=128)
            dma("pool", v, w_d.rearrange("(j p) c -> p j c", p=128)[:, :, c0:c0 + 128], w=["WB.%d" % k])
            return v, "WB.%d" % k

        def projB(wv_, wtok, tb, pb):
            pe([(ps[pb][:, :], wv_[:, j, :], hT[:, j, tb * 512:(tb + 1) * 512], j == 0, j == 7) for j in range(8)],
               [wtok] + tk("hT", tb * 4, tb * 4 + 4), ["ps.%d" % pb])


        triF = Fb(4096, 128)
        triB = Fb(4224, 128)
        rmask = Fb(4352, 512)
        dma("sp", triF, c_triF, w=["triF"])
        dma("sp", triB, c_triB, w=["triB"])
        dma("sp", rmask, c_rmask, w=["rmask"])
        qs, fT, kT, Pc, eA = [Fb(i * 512, 512) for i in range(5)]
        Sf = [Fb(4864, 1152), Fb(2560, 1152)]
        ring["base"], ring["n"] = 7168, 5
        pt = "b0"

        def rbuf(par, off, n=512):
            return Rb(par * 3584 + off, n)

        blocks = [(hh, di, bi, tb) for hh in range(4) for di in range(2)
                  for bi, tb in enumerate([0, 1, 2, 3] if di == 0 else [3, 2, 1, 0])]
        ctxs = {}
        wcur = {}

        def projs(k):
            hh, di, bi, tb = blocks[k]
            if bi == 0:
                wcur[(hh, di)] = (wslice(w_in_d, 0 + hh * 128), wslice(w_in_d, 512 + hh * 128),
                                  wslice(w_in_d, (1536 if di == 0 else 2048) + hh * 128))
            (wq_v, wq_t), (wi_v, wi_t), (wf_v, wf_t) = wcur[(hh, di)]
            projB(wq_v, wq_t, tb, 6)
            projB(wf_v, wf_t, tb, 7)
            projB(wi_v, wi_t, tb, k % 2)

        def stageA1(k):
            hh, di, bi, tb = blocks[k]
            par = k % 2
            rt = "r%d" % par
            lbc = cols[:, 24 + di * 4 + hh:24 + di * 4 + hh + 1]
            omlc = cols[:, 40 + di * 4 + hh:40 + di * 4 + hh + 1]
            qin, kin = rbuf(par, 0), rbuf(par, 512)
            koTM = rbuf(par, 1024).rearrange("p (a c) -> p a c", c=128)
            att = rbuf(par, 1536)
            vTM = rbuf(par, 2048).rearrange("p (a c) -> p a c", c=128)
            SentF = rbuf(par, 2560, 1024).rearrange("p (i c) -> p i c", c=128)
            dec = cols[:, 32:40] if par == 0 else cols[:, 56:64]
            ctxs[k] = dict(qin=qin, kin=kin, koTM=koTM, att=att, vTM=vTM, SentF=SentF, dec=dec)
            A(lambda e: e.activation(qs, ps[6][:, :], AF.Silu), ["ps.6"], [pt + "qs"])
            yield
            A(lambda e: e.activation(fT, ps[7][:, :], AF.Tanh, scale=0.5), ["ps.7"], [pt + "fT"])
            yield
            V(lambda e: e.tensor_scalar(fT, fT, omlc, lbc, ALU.mult, ALU.add), [pt + "fT", "c_lb", "c_oml"], [pt + "fT"])
            yield
            V(lambda e: e.tensor_scalar(kT, fT, -1.0, 1.0, ALU.mult, ALU.add), [pt + "fT"], [pt + "kT"])
            yield
            A(lambda e: e.activation(fT, fT, AF.Ln), [pt + "fT"], [pt + "fT"])
            yield
            V(lambda e: e.tensor_tensor_scan(Pc, rmask, fT, 0.0, ALU.mult, ALU.add), [pt + "fT", "rmask"], [pt + "Pc"])
            yield
            P3 = Pc.rearrange("p (n c) -> p n c", c=64)
            f3 = fT.rearrange("p (n c) -> p n c", c=64)
            tot = P3[:, :, 63:64]
            totb = tot.broadcast_to([128, 8, 64])
            A(lambda e: e.activation(dec.rearrange("p (n o) -> p n o", o=1), tot, AF.Exp), [pt + "Pc"], [rt + "dec"])
            yield
            eA3 = eA.rearrange("p (n c) -> p n c", c=64)
            if di == 0:
                A(lambda e: e.activation(eA, Pc, AF.Exp), [pt + "Pc"], [pt + "eA"])
                yield
                V(lambda e: e.tensor_tensor(qin, qs, eA, ALU.mult), [pt + "qs", pt + "eA"], [rt + "qin"])
                yield
                A(lambda e: e.activation(eA, Pc, AF.Exp, scale=-1.0), [pt + "Pc", rt + "qin"], [pt + "eA"])
                yield
                V(lambda e: e.tensor_tensor(kin, kT, eA, ALU.mult), [pt + "kT", pt + "eA"], [rt + "kin"])
                yield
                V(lambda e: e.tensor_tensor(f3, totb, P3, ALU.subtract), [pt + "Pc"], [pt + "fT"])
                yield
            else:
                V(lambda e: e.tensor_tensor(fT, Pc, fT, ALU.subtract), [pt + "Pc", pt + "fT"], [pt + "fT"])
                yield
                V(lambda e: e.tensor_tensor(eA3, totb, f3, ALU.subtract), [pt + "Pc", pt + "fT"], [pt + "eA"])
                yield
                A(lambda e: e.activation(Pc, eA, AF.Exp), [pt + "eA", rt + "dec"], [pt + "Pc"])
                yield
                V(lambda e: e.tensor_tensor(qin, qs, Pc, ALU.mult), [pt + "qs", pt + "Pc"], [rt + "qin"])
                yield
                A(lambda e: e.activation(Pc, eA, AF.Exp, scale=-1.0), [pt + "eA", rt + "qin"], [pt + "Pc"])
                yield
                V(lambda e: e.tensor_tensor(kin, kT, Pc, ALU.mult), [pt + "kT", pt + "Pc"], [rt + "kin"])
                yield
            if k + 1 < len(blocks):
                projs(k + 1)
                yield
            A(lambda e: e.activation(fT, fT, AF.Exp), [pt + "fT"], [pt + "fT"])
            yield
            V(lambda e: e.tensor_tensor(kT, kT, fT, ALU.mult), [pt + "kT", pt + "fT", rt + "kin"], [pt + "kT"])
            yield
            A(lambda e: e.copy(eA, ps[k % 2][:, :]), ["ps.%d" % (k % 2), rt + "kin", rt + "qin"], [pt + "eA"])
            yield
        def stageA2(k):
            par = k % 2
            rt = "r%d" % par
            koTM, vTM = ctxs[k]["koTM"], ctxs[k]["vTM"]
            o3 = ps[5][:, :].rearrange("p (a c) -> p a c", c=128)
            petr([(o3[:, a, :], kT[:, a * 128:(a + 1) * 128], ident[:, :]) for a in range(4)], [pt + "kT", "ident"], ["ps.5"])
            A(lambda e: e.copy(koTM, o3), ["ps.5"], [rt + "koTM"])
            o3b = ps[2][:, :].rearrange("p (a c) -> p a c", c=128)
            petr([(o3b[:, a, :], eA[:, a * 128:(a + 1) * 128], ident[:, :]) for a in range(4)], [pt + "eA", "ident"], ["ps.2"])
            V(lambda e: e.tensor_copy(vTM, o3b), ["ps.2"], [rt + "vTM"])

        def stageB(k):
            hh, di, bi, tb = blocks[k]
            par = k % 2
            rt = "r%d" % par
            c = ctxs.pop(k)
            qin, kin, koTM, att, vTM, SentF, dec = c["qin"], c["kin"], c["koTM"], c["att"], c["vTM"], c["SentF"], c["dec"]
            tri = triF if di == 0 else triB
            tritok = "triF" if di == 0 else "triB"
            for sc in range(4):
                pe([(ps[5][:, sc * 128:(sc + 1) * 128], kin[:, sc * 128:(sc + 1) * 128], qin[:, sc * 128:(sc + 1) * 128], True, True)],
                   [rt + "kin", rt + "qin"], ["ps.5"])
                yield
            a3 = att.rearrange("p (a c) -> p a c", c=128)
            p53 = ps[5][:, :].rearrange("p (a c) -> p a c", c=128)
            trib = tri[:, None, :].broadcast_to([128, 4, 128])
            V(lambda e: e.tensor_tensor(a3, p53, trib, ALU.mult), ["ps.5", tritok], [rt + "att"])
            yield
            for n in range(8):
                a, r0 = n // 2, (n % 2) * 64
                pbk = 3 + n % 2
                pe([(ps[pbk][:, a * 128:(a + 1) * 128], koTM[r0:r0 + 64, a, :], vTM[r0:r0 + 64, a, :], True, True)],
                   [rt + "koTM", rt + "vTM"], ["ps.%d" % pbk])
                yield
            S = Sf[par].rearrange("p (i c) -> p i c", c=128)
            Sprev = Sf[1 - par].rearrange("p (i c) -> p i c", c=128)
            corder = list(range(8)) if di == 0 else list(range(7, -1, -1))
            if bi == 0:
                V(lambda e: e.memset(S[:, 0, :], 0.0), [], ["S%d_0" % par])
                yield
            else:
                V(lambda e: e.tensor_copy(S[:, 0, :], Sprev[:, 8, :]), ["S%d_8" % (1 - par)], ["S%d_0" % par])
                yield
            for i, n in enumerate(corder):
                pbk = 3 + n % 2
                dl = ps[pbk][:, (n // 2) * 128:(n // 2 + 1) * 128]
                V(lambda e, i=i, n=n, dl=dl: e.scalar_tensor_tensor(S[:, i + 1, :], S[:, i, :], dec[:, n:n + 1], dl, ALU.mult, ALU.add),
                  ["S%d_%d" % (par, i), rt + "dec", "ps.%d" % pbk], ["S%d_%d" % (par, i + 1)])
                yield
            A(lambda e: e.copy(SentF, S[:, 0:8, :]), ["S%d_%d" % (par, i) for i in range(8)], [rt + "SentF"])
            yield
            mms = []
            for sc in range(4):
                mms.append((ps[2][:, sc * 128:(sc + 1) * 128], vTM[:, sc, :], a3[:, sc, :], True, False))
                for cch in range(2):
                    n = sc * 2 + cch
                    i = corder.index(n)
                    mms.append((ps[2][:, n * 64:(n + 1) * 64], SentF[:, i, :], qin[:, n * 64:(n + 1) * 64], False, cch == 1))
            pe(mms, [rt + "vTM", rt + "att", rt + "SentF", rt + "qin"], ["ps.2"])
            yield
            dstc = catT[:, hh, tb * 512:(tb + 1) * 512]
            ctoks = tk("cat%d" % hh, tb, tb + 1)
            if di == 0:
                A(lambda e: e.copy(dstc, ps[2][:, :]), ["ps.2"], ctoks)
                yield
            else:
                V(lambda e: e.tensor_tensor(dstc, dstc.bitcast(F32), ps[2][:, :], ALU.add), ["ps.2"] + ctoks, ctoks)
                yield

        def epilogue(hh, par):
            rt = "r%d" % par
            wg_v, wg_t = wslice(w_in_d, 1024 + hh * 128)
            sgbs = [qs, fT, Pc, eA]
            stoks = [pt + "qs", pt + "fT", pt + "Pc", pt + "eA"]
            sqs = [rbuf(par, 2048), rbuf(par, 1536)]
            sqtk = [rt + "vTM", rt + "att"]
            t1 = kT
            gcol = cols[:, 16 + hh:17 + hh]
            for tb in range(4):
                pb = 2 if tb % 2 == 0 else 5
                projB(wg_v, wg_t, tb, pb)
                A(lambda e, tb=tb, pb=pb: e.activation(sgbs[tb], ps[pb][:, :], AF.Silu), ["ps.%d" % pb], [stoks[tb]])
            for tb in range(4):
                oc = catT[:, hh, tb * 512:(tb + 1) * 512]
                ctoks = tk("cat%d" % hh, tb, tb + 1)
                sq = sqs[tb % 2]
                pz = 3 + tb % 2
                A(lambda e, oc=oc, sq=sq: e.activation(sq, oc.bitcast(F32), AF.Square), ctoks, [sqtk[tb % 2]])
                pe([(ps[pz][:, :], onesR[:, :], sq, True, True)], [sqtk[tb % 2], "onesR"], ["ps.%d" % pz])
                V(lambda e, pz=pz: e.tensor_scalar(t1, ps[pz][:, :], 128.0 * RMS_EPS, None, ALU.add), ["ps.%d" % pz], [pt + "kT"])
                A(lambda e: e.activation(t1, t1, AF.Ln), [pt + "kT"], [pt + "kT"])
                A(lambda e: e.activation(t1, t1, AF.Exp, scale=-0.5), [pt + "kT"], [pt + "kT"])
                V(lambda e, tb=tb: e.scalar_tensor_tensor(t1, t1, gcol, sgbs[tb], ALU.mult, ALU.mult), [pt + "kT", stoks[tb], "c_hgn"], [pt + "kT"])
                V(lambda e, oc=oc: e.tensor_tensor(oc, oc.bitcast(F32), t1, ALU.mult), [pt + "kT"] + ctoks, ctoks)

        def run_interleaved(*gens):
            gens = [g for g in gens if g is not None]
            while gens:
                for g in list(gens):
                    try:
                        next(g)
                    except StopIteration:
                        gens.remove(g)

        projs(0)
        run_interleaved(stageA1(0))
        stageA2(0)
        for k in range(len(blocks)):
            run_interleaved(stageA1(k + 1) if k + 1 < len(blocks) else None, stageB(k))
            if k + 1 < len(blocks):
                stageA2(k + 1)
            hh, di, bi, tb = blocks[k]
            if di == 1 and bi == 3:
                epilogue(hh, k % 2)
        P.barrier()
        if stop == 1:
            P.add("pool", lambda e: e.nop(), [], ["DUMP"])
            dump_and_finish()
            P.emit(nc)
            return nc

        ring["base"], ring["n"] = 8704, 3
        wctr[0] = 0
        ropeC = Fb(0, 2048)
        ropeS = Fb(2048, 2048)
        tmpq = Fb(5120, 512)
        tmpvv = Fb(5632, 512)
        sqb = Rb(8192, 512)
        onesF = Fb(6144, 128)
        dma("sp", onesF, c_ones, w=["onesF"])
        dma("sp", ropeC, c_ropeC, w=["ropeC"])
        dma("sp", ropeS, c_ropeS, w=["ropeS"])
        qT = Rb(0, 2048)
        kTt = Rb(2048, 2048)
        VTM = Rb(4096, 2048).rearrange("p (a c) -> p a c", c=128)
        Er = [Rb(6144 + i * 512, 512) for i in range(4)]
        for hh in range(4):
            pstep = 0
            for which, dst, base, swb in (("q", qT, 2560, 0), ("k", kTt, 3072, 512)):
                w1, t1_ = wslice(w_in_d, base + hh * 128)
                w2, t2_ = wslice(w_sw_d, swb + hh * 128)
                for tb in range(4):
                    pa = 2 * (pstep % 2)
                    pb_ = pa + 1
                    tmpb = [tmpq, tmpvv][pstep % 2]
                    ttk = ["tmpq", "tmpvv"][pstep % 2]
                    pstep += 1
                    projB(w1, t1_, tb, pa)
                    projB(w2, t2_, tb, pb_)
                    d_ = dst[:, tb * 512:(tb + 1) * 512]
                    V(lambda e, tmpb=tmpb, tb=tb, pb_=pb_: e.tensor_tensor(tmpb, ps[pb_][:, :], ropeS[:, tb * 512:(tb + 1) * 512], ALU.mult),
                      ["ps.%d" % pb_, "ropeS"], [ttk])
                    V(lambda e, d_=d_, tb=tb, pa=pa: e.tensor_tensor(d_, ps[pa][:, :], ropeC[:, tb * 512:(tb + 1) * 512], ALU.mult),
                      ["ps.%d" % pa, "ropeC"], ["%sT.%d" % (which, tb)])
                    V(lambda e, d_=d_, tmpb=tmpb: e.tensor_tensor(d_, d_.bitcast(F32), tmpb, ALU.add),
                      [ttk, "%sT.%d" % (which, tb)], ["%sT.%d" % (which, tb)])
            wv_v, wv_t = wslice(w_in_d, 3584 + hh * 128)
            for tb in range(4):
                pv = 4 + tb % 2
                pt_ = 6 + tb % 2
                tmpb = [Fb(4096, 512), Fb(4608, 512)][tb % 2]
                ttk = ["r0", "r1"][tb % 2]
                projB(wv_v, wv_t, tb, pv)
                A(lambda e, tmpb=tmpb, pv=pv: e.copy(tmpb, ps[pv][:, :]), ["ps.%d" % pv], [ttk])
                o3 = ps[pt_][:, :].rearrange("p (a c) -> p a c", c=128)
                petr([(o3[:, a, :], tmpb[:, a * 128:(a + 1) * 128], ident[:, :]) for a in range(4)], [ttk, "ident"], ["ps.%d" % pt_])
                V(lambda e, tb=tb, o3=o3: e.tensor_copy(VTM[:, tb * 4:tb * 4 + 4, :], o3), ["ps.%d" % pt_], ["VTM.%d" % tb])
            tails = []
            for qb in range(4):
                steps = [(j, kp) for j in range(2) for kp in range(8)]
                stb = [(0, 1), (4, 5)]
                Zacc = [tmpq, tmpvv]
                ztok = ["tmpq", "tmpvv"]

                def st_mm(i):
                    j, kp = steps[i]
                    r0 = j * 64
                    bb = stb[i % 2]
                    pe([(ps[bb[h2]][:, :], kTt[r0:r0 + 64, (2 * kp + h2) * 128:(2 * kp + h2 + 1) * 128], qT[r0:r0 + 64, qb * 512:(qb + 1) * 512], True, True)
                        for h2 in range(2)], ["kT.%d" % (kp // 2), "qT.%d" % qb], ["ps.%d" % bb[0], "ps.%d" % bb[1]])

                st_mm(0)
                for i, (j, kp) in enumerate(steps):
                    if i + 1 < len(steps):
                        st_mm(i + 1)
                    bb = stb[i % 2]
                    ebs = [Er[(i % 2) * 2 + h2] for h2 in range(2)]
                    etk = ["E%d" % ((i % 2) * 2 + h2) for h2 in range(2)]
                    for h2 in range(2):
                        A(lambda e, Eb=ebs[h2], pb=bb[h2]: e.activation(Eb, ps[pb][:, :], AF.Exp, scale=0.125), ["ps.%d" % bb[h2]], [etk[h2]])
                    pe([(ps[2 + j][:, :], VTM[:, 2 * kp + h2, :], ebs[h2], (2 * kp + h2) == 0, (2 * kp + h2) == 15) for h2 in range(2)],
                       ["VTM.%d" % (kp // 2)] + etk, ["ps.%d" % (2 + j)])
                    for h2 in range(2):
                        if 2 * kp + h2 == 0:
                            V(lambda e, Z=Zacc[j], Eb=ebs[h2]: e.tensor_copy(Z, Eb.bitcast(F32)), [etk[h2]], [ztok[j]])
                        else:
                            V(lambda e, Z=Zacc[j], Eb=ebs[h2]: e.tensor_tensor(Z, Z, Eb.bitcast(F32), ALU.add), [etk[h2], ztok[j]], [ztok[j]])
                    if i in (1, 2) and tails:
                        tails.pop(0)()
                for j in range(2):
                    pe([(ps[6 + j][:, :], onesF, Zacc[j], True, True)], ["onesF", ztok[j]], ["ps.%d" % (6 + j)])
                r_0 = Fb(4096, 512)
                r_1 = Fb(4608, 512)
                A(lambda e, r_0=r_0: e.activation(r_0, ps[6][:, :], AF.Ln), ["ps.6"], ["r0"])
                A(lambda e, r_1=r_1: e.activation(r_1, ps[7][:, :], AF.Ln), ["ps.7"], ["r1"])
                A(lambda e, r_0=r_0: e.activation(r_0, r_0, AF.Exp, scale=-1.0), ["r0"], ["r0"])
                A(lambda e, r_1=r_1: e.activation(r_1, r_1, AF.Exp, scale=-1.0), ["r1"], ["r1"])
                V(lambda e, r_0=r_0: e.tensor_tensor(r_0, r_0, ps[2][:, :], ALU.mult), ["r0", "ps.2"], ["r0"])
                V(lambda e, r_1=r_1: e.tensor_tensor(r_1, r_1, ps[3][:, :], ALU.mult), ["r1", "ps.3"], ["r1"])
                def tail1(r_0=r_0, r_1=r_1):
                    V(lambda e: e.scalar_tensor_tensor(r_0, r_1, cols[:, 21:22], r_0, ALU.mult, ALU.add), ["r0", "r1", "c_nlam"], ["r0"])
                    A(lambda e: e.activation(sqb, r_0, AF.Square), ["r0"], ["sqb"])
                    pe([(ps[6][:, :], onesR[:, :], sqb, True, True)], ["sqb", "onesR"], ["ps.6"])

                def tail2(r_0=r_0, r_1=r_1, qb=qb, hh=hh):
                    V(lambda e: e.tensor_scalar(r_1, ps[6][:, :], 128.0 * RMS_EPS, None, ALU.add), ["ps.6", "r1"], ["r1"])
                    A(lambda e: e.activation(r_1, r_1, AF.Ln), ["r1"], ["r1"])
                    A(lambda e: e.activation(r_1, r_1, AF.Exp, scale=-0.5), ["r1"], ["r1"])
                    oc = catT[:, 4 + hh, qb * 512:(qb + 1) * 512]
                    V(lambda e: e.scalar_tensor_tensor(oc, r_0, cols[:, 20:21], r_1, ALU.mult, ALU.mult),
                      ["r0", "r1", "c_sub"], tk("cat%d" % (4 + hh), qb, qb + 1))

                tails += [tail1, tail2]
            while tails:
                tails.pop(0)()
        P.barrier()
        if stop == 2:
            P.add("pool", lambda e: e.nop(), [], ["DUMP"])
            dump_and_finish()
            P.emit(nc)
            return nc

        def load_wfull(w_d):
            v = SR[:, 0:8192].rearrange("p (j c) -> p j c", c=1024)
            for q4 in range(4):
                dma("pool", v[:, 2 * q4:2 * q4 + 2, :], w_d.rearrange("(j p) c -> p j c", p=128)[:, 2 * q4:2 * q4 + 2, :],
                    w=["WB.%d" % (2 * q4), "WB.%d" % (2 * q4 + 1)])
            return v

        def proj_ln(wfull, tm_dst=None):
            aI4 = SR[:, 8192:10240].rearrange("p (j c) -> p j c", c=512)
            dma("pool", SR[:, 8192:10240], c_aI4, w=["aI4"])

            def emit_mm(tc):
                p0 = 2 * (tc % 2)
                for b in range(2):
                    mms = []
                    for jj in range(4):
                        mms.append((ps[p0 + b][:, :], hT[:, 4 * b + jj, tc * 128:(tc + 1) * 128], aI4[:, jj, :], jj == 0, False))
                    for k in range(8):
                        mms.append((ps[p0 + b][:, :], catT[:, k, tc * 128:(tc + 1) * 128], wfull[:, k, b * 512:(b + 1) * 512], False, k == 7))
                    pe(mms, ["hT.%d" % tc, "aI4"] + ["cat%d.%d" % (k, tc // 4) for k in range(8)] + tk("WB", 0, 8), ["ps.%d" % (p0 + b)])

            emit_mm(0)
            for tc in range(16):
                p0 = 2 * (tc % 2)
                if tc + 1 < 16:
                    emit_mm(tc + 1)
                xb = xt[tc % 2]
                ln_chunk(ps[p0][:, :], ps[p0 + 1][:, :], ["ps.%d" % p0, "ps.%d" % (p0 + 1)], xb[:, :], ["xt%d" % (tc % 2)], xb[:, :], "xt%d" % (tc % 2), "ln")
                tm_to_fm(xb, ["xt%d" % (tc % 2)], tc, (4 + p0, 5 + p0))

        wfull = load_wfull(w_mix_d)
        load_ln(1)
        proj_ln(wfull)
        P.barrier()
        if stop == 3:
            P.add("pool", lambda e: e.nop(), [], ["DUMP"])
            dump_and_finish()
            P.emit(nc)
            return nc

        ring["base"], ring["n"] = 9216, 3
        wctr[0] = 0
        memTM = Fb(0, 2048).rearrange("p (a c) -> p a c", c=1024)
        memT = Rb(0, 2048).rearrange("p (j m) -> p j m", m=256)
        KmT = Rb(2048, 2048).rearrange("p (j m) -> p j m", m=256)
        Vm = Rb(4096, 2048).rearrange("p (a c) -> p a c", c=1024)
        qxT = Rb(6144, 2048).rearrange("p (a t) -> p a t", t=1024)
        Ex = [Rb(8192 + i * 512, 512) for i in range(2)]
        tmpv4 = Fb(4608, 512)
        for a in range(2):
            dma("sp", memTM[:, a, :], mem_d[a * 128:(a + 1) * 128, :], w=["memTM%d" % a])
        for a in range(2):
            for b in range(2):
                o3 = ps[6 + b][:, :].rearrange("p (q c) -> p q c", c=128)
                petr([(o3[:, q, :], memTM[:, a, (4 * b + q) * 128:(4 * b + q + 1) * 128], ident[:, :]) for q in range(4)],
                     ["memTM%d" % a, "ident"], ["ps.%d" % (6 + b)])
                copy_any(memT[:, 4 * b:4 * b + 4, a * 128:(a + 1) * 128], o3, ["ps.%d" % (6 + b)], ["memT"])
        for ec in range(8):
            wv_, wt_ = wslice(wk_d, ec * 128)
            pk = 6 + ec % 2
            pe([(ps[pk][:, 0:256], wv_[:, j, :], memT[:, j, :], j == 0, j == 7) for j in range(8)], [wt_, "memT"], ["ps.%d" % pk])
            copy_any(KmT[:, ec, :], ps[pk][:, 0:256], ["ps.%d" % pk], ["KmT.%d" % ec])
        tmpvs = [tmpv4, Fb(5120, 512)]
        for ec in range(8):
            wv_, wt_ = wslice(wv_d, ec * 128)
            pv_, pt_ = (6, 7) if ec % 2 == 0 else (4, 5)
            tmpv = tmpvs[ec % 2]
            tvk = "tmpv4_%d" % (ec % 2)
            pe([(ps[pv_][:, 0:256], wv_[:, j, :], memT[:, j, :], j == 0, j == 7) for j in range(8)], [wt_, "memT"], ["ps.%d" % pv_])
            A(lambda e, tmpv=tmpv, pv_=pv_: e.copy(tmpv[:, 0:256], ps[pv_][:, 0:256]), ["ps.%d" % pv_], [tvk])
            petr([(ps[pt_][:, a * 128:(a + 1) * 128], tmpv[:, a * 128:(a + 1) * 128], ident[:, :]) for a in range(2)], [tvk, "ident"], ["ps.%d" % pt_])
            V(lambda e, ec=ec, pt_=pt_: e.tensor_copy(Vm[:, :, ec * 128:(ec + 1) * 128], ps[pt_][:, 0:256].rearrange("p (a c) -> p a c", c=128)),
              ["ps.%d" % pt_], ["Vm.%d" % ec])
        xoT = catT
        qx = [qxT, Rb(0, 2048).rearrange("p (a t) -> p a t", t=1024)]

        def qproj_gen(m):
            hx, half = divmod(m, 2)
            q = qx[m % 2]
            extra = ["memT"] if m % 2 == 1 else []
            for ec in range(2):
                wv_, wt_ = wslice(wq_d, hx * 256 + ec * 128)
                for tbl in range(2):
                    tb = half * 2 + tbl
                    pb = 6 + tbl
                    projB(wv_, wt_, tb, pb)
                    yield
                    copy_any(q[:, ec, tbl * 512:(tbl + 1) * 512], ps[pb][:, :], ["ps.%d" % pb], ["qx%d_%d.%d" % (m % 2, ec, tbl)] + extra)
                    yield

        def attn_gen(m):
            hx, half = divmod(m, 2)
            q = qx[m % 2]
            qk = "qx%d_" % (m % 2)
            for tbl in range(2):
                tb = half * 2 + tbl
                for mc in range(2):
                    pe([(ps[mc][:, :], KmT[:, hx * 2 + ec, mc * 128:(mc + 1) * 128], q[:, ec, tbl * 512:(tbl + 1) * 512], ec == 0, ec == 1)
                        for ec in range(2)], ["KmT.%d" % (hx * 2), "KmT.%d" % (hx * 2 + 1), qk + "0.%d" % tbl, qk + "1.%d" % tbl], ["ps.%d" % mc])
                    yield
                    A(lambda e, mc=mc: e.activation(Ex[mc], ps[mc][:, :], AF.Exp, scale=1.0 / 16.0), ["ps.%d" % mc], ["Ex%d" % mc])
                    yield
                for ec2 in range(2):
                    pe([(ps[2 + ec2][:, :], Vm[:, mc, hx * 256 + ec2 * 128:hx * 256 + (ec2 + 1) * 128], Ex[mc], mc == 0, mc == 1)
                        for mc in range(2)], ["Vm.%d" % (hx * 2 + ec2), "Ex0", "Ex1"], ["ps.%d" % (2 + ec2)])
                    yield
                pe([(ps[4][:, :], onesR[:, :], Ex[mc], mc == 0, mc == 1) for mc in range(2)], ["onesR", "Ex0", "Ex1"], ["ps.4"])
                yield
                rz = Fb(4096, 512)
                A(lambda e, rz=rz: e.activation(rz, ps[4][:, :], AF.Ln), ["ps.4"], ["rz"])
                yield
                A(lambda e, rz=rz: e.activation(rz, rz, AF.Exp, scale=-1.0), ["rz"], ["rz"])
                yield
                for ec2 in range(2):
                    oc = xoT[:, hx * 2 + ec2, tb * 512:(tb + 1) * 512]
                    V(lambda e, oc=oc, rz=rz, ec2=ec2: e.tensor_tensor(oc, ps[2 + ec2][:, :], rz, ALU.mult),
                      ["ps.%d" % (2 + ec2), "rz"], tk("cat%d" % (hx * 2 + ec2), tb, tb + 1))
                    yield

        interleave(qproj_gen(0))
        for m in range(8):
            interleave(qproj_gen(m + 1) if m + 1 < 8 else None, attn_gen(m))
        P.barrier()
        wfull = load_wfull(wo_d)
        load_ln(2)
        proj_ln(wfull)
        P.barrier()
        if stop == 4:
            P.add("pool", lambda e: e.nop(), [], ["DUMP"])
            dump_and_finish()
            P.emit(nc)
            return nc

        h2TM = RB[:, :].rearrange("p (a c) -> p a c", c=1024)
        wr = Fb(4096, 128).rearrange("p (j c) -> p j c", c=16)
        dma("sp", wr, wr_d.rearrange("(j p) c -> p j c", p=128), w=["wr"])
        affT = FA[0:16, 0:2048]
        mskT = FA[0:16, 2048:4096]
        posT = FA[0:16, 4224:6272]
        mgT = affT
        ones16T = FA[0:16, 6272:6288]
        bs = FA[0:16, 6288:6304]
        iotaf = smallp[:, 0:256]
        posTM = smallp[:, 256:512]
        mgTM = smallp[:, 512:768]
        dma("sp", iotaf, c_iotaf, w=["iotaf"])
        V(lambda e: e.memset(ones16T, 1.0), [], ["ones16T"])
        for tb in range(4):
            pe([(ps[tb][0:16, :], wr[:, j, :], hT[:, j, tb * 512:(tb + 1) * 512].bitcast(F32), j == 0, j == 7) for j in range(8)],
               ["wr"] + tk("hT", tb * 4, tb * 4 + 4), ["ps.%d" % tb])
            A(lambda e, tb=tb: e.activation(affT[:, tb * 512:(tb + 1) * 512], ps[tb][0:16, :], AF.Exp), ["ps.%d" % tb], ["affT"])
        for tb in range(4):
            pe([(ps[4][0:16, :], ones16T[:, 0:16], affT[:, tb * 512:(tb + 1) * 512], True, True)], ["affT", "ones16T"], ["ps.4"])
            A(lambda e, tb=tb: e.activation(mskT[:, tb * 512:(tb + 1) * 512], ps[4][0:16, :], AF.Ln), ["ps.4"], ["mskT"])
            A(lambda e, tb=tb: e.activation(mskT[:, tb * 512:(tb + 1) * 512], mskT[:, tb * 512:(tb + 1) * 512], AF.Exp, scale=-1.0), ["mskT"], ["mskT"])
        V(lambda e: e.tensor_tensor(affT, affT, mskT, ALU.mult), ["affT", "mskT"], ["affT"])
        V(lambda e: e.memset(bs[:, 0:1], 0.0), [], ["bs"])
        for it in range(NITER):
            wi = 2.0 ** -(it + 1)
            V(lambda e, wi=wi: e.tensor_scalar(bs[:, 2:3], bs[:, 0:1], wi, None, ALU.add), ["bs"], ["bs"])
            V(lambda e: e.tensor_scalar(mskT, affT, bs[:, 2:3], 0.0, ALU.is_ge, ALU.add, accum_out=bs[:, 3:4]), ["bs", "affT"], ["mskT", "bs"])
            V(lambda e, wi=wi: e.tensor_scalar(bs[:, 4:5], bs[:, 3:4], float(CAP), wi, ALU.is_ge, ALU.mult), ["bs"], ["bs"])
            V(lambda e: e.tensor_tensor(bs[:, 0:1], bs[:, 0:1], bs[:, 4:5], ALU.add), ["bs"], ["bs"])
        V(lambda e: e.tensor_scalar(mskT, affT, bs[:, 0:1], None, ALU.is_ge), ["bs", "affT"], ["mskT"])
        V(lambda e: e.tensor_tensor_scan(posT, ones16T[:, 0:1].broadcast_to([16, T]), mskT, 0.0, ALU.mult, ALU.add), ["mskT", "ones16T"], ["posT"])
        V(lambda e: e.tensor_tensor(posT, posT, mskT, ALU.mult), ["posT", "mskT"], ["posT"])
        V(lambda e: e.tensor_scalar(posT, posT, -1.0, None, ALU.add), ["posT"], ["posT"])
        V(lambda e: e.tensor_tensor(mgT, mskT, affT, ALU.mult), ["mskT", "affT"], ["affT", "mgT"])
        for tc in range(16):
            for b in range(2):
                o3 = ps[6 + b][:, :].rearrange("p (q c) -> p q c", c=128)
                petr([(o3[:, q, :], hT[:, 4 * b + q, tc * 128:(tc + 1) * 128].bitcast(F32), ident[:, :]) for q in range(4)],
                     ["hT.%d" % tc, "ident"], ["ps.%d" % (6 + b)])
                A(lambda e, tc=tc, b=b: e.copy(h2TM[:, tc, b * 512:(b + 1) * 512], ps[6 + b][:, :]), ["ps.%d" % (6 + b)], ["h2TM.%d.%d" % (tc, b)])
        p3 = ps[5][:, 0:256].rearrange("p (a c) -> p a c", c=16)
        petr([(p3[:, tc, :], posT[:, tc * 128:(tc + 1) * 128], ident[0:16, 0:16]) for tc in range(16)], ["posT", "ident"], ["ps.5"])
        V(lambda e: e.tensor_copy(posTM, ps[5][:, 0:256]), ["ps.5"], ["posTM"])
        p3b = ps[4][:, 0:256].rearrange("p (a c) -> p a c", c=16)
        petr([(p3b[:, tc, :], mgT[:, tc * 128:(tc + 1) * 128], ident[0:16, 0:16]) for tc in range(16)], ["mgT", "ident"], ["ps.4"])
        V(lambda e: e.tensor_copy(mgTM, ps[4][:, 0:256]), ["ps.4"], ["mgTM"])
        acc_w = RA[:, :].rearrange("p (a c) -> p a c", c=1024)
        acc = RA[:, :].bitcast(F32).rearrange("p (a c) -> p a c", c=1024)
        for tc in range(16):
            A(lambda e, tc=tc: e.mul(acc_w[:, tc, :], h2TM[:, tc, :].bitcast(F32), ALPHA),
              ["h2TM.%d.0" % tc, "h2TM.%d.1" % tc] + tk("hT", 0, 16), ["acc.%d.0" % tc, "acc.%d.1" % tc])
        P.barrier()
        if stop == 5:
            P.add("pool", lambda e: e.nop(), [], ["DUMP"])
            dump_and_finish()
            P.emit(nc)
            return nc

        cd3 = Rb(0, 2048).rearrange("p (a c) -> p a c", c=1024)
        xs3 = Rb(2048, 2048).rearrange("p (j c) -> p j c", c=256)
        selp = [Rb(4096 + i * 256, 256) for i in range(2)]
        actT = [Rb(4608 + i * 256, 256) for i in range(2)]
        SelGh = [Rb(5120 + i * 512, 512).rearrange("p (a t) -> p a t", t=256) for i in range(2)]
        xst3 = Fb(0, 2048).rearrange("p (a c) -> p a c", c=1024)
        sgt2 = [Fb(4096, 256), Fb(4864, 256)]
        sgtm = [Fb(4352 + i * 256, 256) for i in range(2)]

        def scatter_items(ex):
            items = []
            for hb in range(8):
                SG = SelGh[hb % 2]
                sgk = "SelG%d" % (hb % 2)

                def prep(hb=hb, SG=SG, sgk=sgk):
                    for t in range(2):
                        tc = hb * 2 + t
                        sm = sgtm[t]
                        V(lambda e, sm=sm, tc=tc: e.tensor_scalar(sm, iotaf, posTM[:, tc * 16 + ex:tc * 16 + ex + 1], mgTM[:, tc * 16 + ex:tc * 16 + ex + 1], ALU.is_equal, ALU.mult),
                          ["iotaf", "posTM", "mgTM"], ["sgtm%d" % t])
                        petr([(ps[6][:, (cc * 2 + t) * 128:(cc * 2 + t + 1) * 128], sm[:, cc * 128:(cc + 1) * 128], ident[:, :]) for cc in range(2)],
                             ["sgtm%d" % t, "ident"], ["ps.6"])
                    copy_any(SG, ps[6][:, :].rearrange("p (a t) -> p a t", t=256), ["ps.6"], [sgk])
                items.append(prep)
                for t in range(2):
                    for db in range(2):
                        def tile(hb=hb, t=t, db=db, SG=SG, sgk=sgk):
                            tc = hb * 2 + t
                            pe([(ps[7][:, :], SG[:, cc, t * 128:(t + 1) * 128], cd3[:, cc, db * 512:(db + 1) * 512], cc == 0, cc == 1)
                                for cc in range(2)], [sgk, "cd.0.%d" % db, "cd.1.%d" % db], ["ps.7"])
                            V(lambda e: e.tensor_tensor(acc_w[:, tc, db * 512:(db + 1) * 512], acc[:, tc, db * 512:(db + 1) * 512], ps[7][:, :], ALU.add),
                              ["ps.7", "acc.%d.%d" % (tc, db)], ["acc.%d.%d" % (tc, db)])
                        items.append(tile)
            return items

        pending = []

        def drain(n):
            for _ in range(n):
                if pending:
                    pending.pop(0)()

        for ex in range(16):
            for tc in range(16):
                sp_ = selp[tc % 2]
                V(lambda e, sp_=sp_, tc=tc, ex=ex: e.tensor_scalar(sp_, iotaf, posTM[:, tc * 16 + ex:tc * 16 + ex + 1], None, ALU.is_equal),
                  ["iotaf", "posTM"], ["selp%d" % (tc % 2)])
                pe([(ps[dch // 2][:, (dch % 2) * 256:(dch % 2 + 1) * 256], h2TM[:, tc, dch * 128:(dch + 1) * 128], sp_,
                     tc == 0 and dch % 2 == 0, tc == 15) for dch in range(8)],
                   ["selp%d" % (tc % 2), "h2TM.%d.0" % tc, "h2TM.%d.1" % tc], tk("ps", 0, 4), skip=True)
            for b4 in range(4):
                copy_any(xs3[:, 2 * b4:2 * b4 + 2, :], ps[b4][:, :].rearrange("p (a c) -> p a c", c=256), ["ps.%d" % b4], ["xsT.%d" % b4])

            def dma_down(fb, ex=ex):
                sset = fb % 2
                dma("pool", SR[:, 10240 + sset * 1024:10240 + (sset + 1) * 1024], wd_d[ex][fb * 128:(fb + 1) * 128, :], w=["WS%dd" % sset])

            def ffn_front(fb, ex=ex):
                sset = fb % 2
                o = 6144 + sset * 2048
                wgs = SR[:, o:o + 1024].rearrange("p (j c) -> p j c", c=128)
                wus = SR[:, o + 1024:o + 2048].rearrange("p (j c) -> p j c", c=128)
                wds = SR[:, 10240 + sset * 1024:10240 + (sset + 1) * 1024]
                wt = "WS%d" % sset
                dma("pool", SR[:, o:o + 1024], wg_d[ex, fb], w=[wt + "g"])
                dma("pool", SR[:, o + 1024:o + 2048], wu_d[ex, fb], w=[wt + "u"])
                if fb >= 1:
                    dma_down(fb - 1)
                pe([(ps[4][:, 0:256], wgs[:, j, :], xs3[:, j, :], j == 0, j == 7) for j in range(8)], [wt + "g"] + ["xsT.%d" % b_ for b_ in range(4)], ["ps.4"])
                drain(1)
                pe([(ps[5][:, 0:256], wus[:, j, :], xs3[:, j, :], j == 0, j == 7) for j in range(8)], [wt + "u"] + ["xsT.%d" % b_ for b_ in range(4)], ["ps.5"])
                sg_ = sgt2[sset]
                at_ = actT[sset]
                A(lambda e, sg_=sg_: e.activation(sg_, ps[4][:, 0:256], AF.Silu), ["ps.4"], ["sgt%d" % sset])
                V(lambda e, at_=at_, sg_=sg_: e.tensor_tensor(at_, sg_, ps[5][:, 0:256], ALU.mult),
                  ["sgt%d" % sset, "ps.5"], ["actT%d" % sset])
                drain(1)

            def ffn_down(fb):
                sset = fb % 2
                wds = SR[:, 10240 + sset * 1024:10240 + (sset + 1) * 1024]
                at_ = actT[sset]
                pe([(ps[cc * 2 + db][:, :], at_[:, cc * 128:(cc + 1) * 128], wds[:, db * 512:(db + 1) * 512], fb == 0, fb == 15)
                    for cc in range(2) for db in range(2)], ["actT%d" % sset, "WS%dd" % sset], tk("ps", 0, 4))
                drain(1)

            ffn_front(0)
            for fb in range(16):
                if fb + 1 < 16:
                    ffn_front(fb + 1)
                else:
                    dma_down(15)
                ffn_down(fb)
            drain(len(pending))
            for cc in range(2):
                for db in range(2):
                    copy_any(cd3[:, cc, db * 512:(db + 1) * 512], ps[cc * 2 + db][:, :], ["ps.%d" % (cc * 2 + db)], ["cd.%d.%d" % (cc, db)])
            pending = scatter_items(ex)
        drain(len(pending))
        load_ln(3)
        def ln3_chain(tc):
            par = tc % 2
            xb = xt[par]
            xk = "xt%d" % par
            yield from ln_chunk_gen(acc[:, tc, 0:512], acc[:, tc, 512:1024], ["acc.%d.0" % tc, "acc.%d.1" % tc], xb[:, :], [xk], xb[:, :], xk, par)
            dma("sp", out_d[tc * 128:(tc + 1) * 128, :], xb[:, :], r=[xk], outp=True)
            yield

        for tc in range(0, 16, 2):
            interleave(ln3_chain(tc), ln3_chain(tc + 1))
        P.emit(nc)
    return nc


def _consts():
    c = {}
    c["c_ident"] = np.eye(128, dtype=np.float32)
    c["c_aI"] = (np.eye(128) * ALPHA).astype(np.float32)
    a4 = np.zeros((128, 4, 512), np.float32)
    for j in range(4):
        a4[np.arange(128), j, j * 128 + np.arange(128)] = ALPHA
    c["c_aI4"] = np.ascontiguousarray(a4.reshape(128, 2048))
    c["c_ones"] = np.ones((128, 128), np.float32)
    s = np.arange(128)[:, None]
    cc = np.arange(128)[None, :]
    same = (s // 64) == (cc // 64)
    c["c_triF"] = (same & (s <= cc)).astype(np.float32)
    c["c_triB"] = (same & (s >= cc)).astype(np.float32)
    rm = np.ones((128, 512), np.float32)
    rm[:, ::64] = 0.0
    c["c_rmask"] = rm
    inv = 1.0 / (500000.0 ** (np.arange(0, 16, 2, dtype=np.float32) / 16.0))
    ang = np.arange(T, dtype=np.float32)[None, :] * inv[:, None]
    C = np.ones((128, T), np.float32)
    S = np.zeros((128, T), np.float32)
    for comp in range(2):
        b = comp * 64
        C[b:b + 8] = np.cos(ang)
        C[b + 8:b + 16] = np.cos(ang)
        S[b:b + 8] = -np.sin(ang)
        S[b + 8:b + 16] = np.sin(ang)
    c["c_ropeC"] = C
    c["c_ropeS"] = S
    c["c_iotaf"] = np.tile(np.arange(256, dtype=np.float32)[None, :], (128, 1))
    return c


def _swap_cols(w_in):
    out = np.empty((D, 1024), np.float32)
    for bi, base in enumerate((2560, 3072)):
        blk = w_in[:, base:base + 512].reshape(D, 8, 64)
        sw = blk.copy()
        sw[:, :, 0:8] = blk[:, :, 8:16]
        sw[:, :, 8:16] = blk[:, :, 0:8]
        out[:, bi * 512:(bi + 1) * 512] = sw.reshape(D, 512)
    return out


def _ffn_layout(w):
    e = w.shape[0]
    v = w.reshape(e, 8, 128, 16, 128).transpose(0, 3, 2, 1, 4)
    return np.ascontiguousarray(v).reshape(e, 16, 128, 1024)


_NC_CACHE = {}


def _prep_inputs(inp):
    f = lambda a: np.ascontiguousarray(np.asarray(a, dtype=np.float32))
    w_in = f(inp["w_in"])[0]
    shared = {
        "w_in": w_in,
        "w_sw": _swap_cols(w_in),
        "w_mix": f(inp["w_mix_out"])[0],
        "xa_wq": f(inp["xa_wq"])[0], "xa_wk": f(inp["xa_wk"])[0], "xa_wv": f(inp["xa_wv"])[0], "xa_wo": f(inp["xa_wo"])[0],
        "w_router": f(inp["w_router"])[0],
        "w_gate": _ffn_layout(f(inp["w_gate"])[0]), "w_up": _ffn_layout(f(inp["w_up"])[0]), "w_down": f(inp["w_down"])[0],
        "ln_g": np.stack([f(inp["emb_ln_g"]), f(inp["ln1_g"])[0], f(inp["ln2_g"])[0], f(inp["ln3_g"])[0]]),
        "ln_b": np.stack([f(inp["emb_ln_b"]), f(inp["ln1_b"])[0], f(inp["ln2_b"])[0], f(inp["ln3_b"])[0]]),
        "hg_lb": f(inp["hg_lb_logits"]).reshape(16, 128),
        "hg_norm_g": f(inp["hg_norm_g"]).reshape(4, 128),
        "da_subln_g": f(inp["da_subln_g"]).reshape(1, 128),
        "da_lam": np.stack([f(inp["da_lambda_q1"])[0], f(inp["da_lambda_k1"])[0], f(inp["da_lambda_q2"])[0], f(inp["da_lambda_k2"])[0]]),
    }
    shared.update(_consts())
    x = f(inp["x"])
    mem = f(inp["mem"])
    return [dict(shared, x=x[b], mem=mem[b]) for b in range(8)]


def kernel(**inputs):
    if "full" not in _NC_CACHE:
        _NC_CACHE["full"] = build(None)
    nc = _NC_CACHE["full"]
    in_maps = _prep_inputs(inputs)
    res = run_bass_kernel_spmd(nc, in_maps, core_ids=list(range(8)))
    return np.stack([np.asarray(r["out"], dtype=np.float32) for r in res.results], axis=0)
```

```python
import contextlib
import math
import numpy as np
import concourse.bass as bass
import concourse.mybir as mybir
from concourse.bass_utils import run_bass_kernel_spmd

F32 = mybir.dt.float32
F32R = mybir.dt.float32r
AF = mybir.ActivationFunctionType
ALU = mybir.AluOpType
AX = mybir.AxisListType

T = 2048
D = 1024
ALPHA = 2.0 ** 0.25
LN_EPS = 1e-5
RMS_EPS = 1e-6
LAMBDA_INIT = 0.8 - 0.6 * math.exp(0.0)
CAP = 256
NITER = 28
import os
_DBG_SKIP64 = os.environ.get('DBG_SKIP64') == '1'
ENGS = ["pe", "act", "dve", "pool", "sp"]


class Op:
    __slots__ = ("i", "eng", "fn", "deps", "dma", "out")

    def __init__(self, i, eng, fn, deps, dma, out):
        self.i, self.eng, self.fn, self.deps, self.dma, self.out = i, eng, fn, deps, dma, out


class Prog:
    def __init__(self):
        self.ops = []
        self.lastw = {}
        self.readers = {}
        self.since_barrier = []
        self.barrier_op = None

    def add(self, eng, fn, r=(), w=(), dma=False, out=False):
        i = len(self.ops)
        deps = set()
        if self.barrier_op is not None:
            deps.add(self.barrier_op)
        for t in r:
            if t in self.lastw:
                deps.add(self.lastw[t])
        for t in w:
            if t in self.lastw:
                deps.add(self.lastw[t])
            deps.update(self.readers.get(t, ()))
        for t in r:
            self.readers.setdefault(t, []).append(i)
        for t in w:
            self.lastw[t] = i
            self.readers[t] = []
        deps.discard(i)
        self.ops.append(Op(i, eng, fn, deps, dma, out))
        self.since_barrier.append(i)
        return i

    def barrier(self):
        i = len(self.ops)
        deps = set(self.since_barrier)
        self.ops.append(Op(i, "pool", lambda e: e.nop(), deps, False, False))
        self.since_barrier = []
        self.barrier_op = i

    def emit(self, nc, n_dma_sems=10):
        ops = self.ops
        n = len(ops)
        needs = [False] * n
        for op in ops:
            for d in op.deps:
                needs[d] = True
        with contextlib.ExitStack() as st:
            csem = {e: st.enter_context(nc.semaphore("c_" + e)) for e in ENGS}
            dsems = {e: [st.enter_context(nc.semaphore("d_%s%d" % (e, k))) for k in range(n_dma_sems)]
                     for e in ("sp", "act", "pool")}
            duse = {e: [0] * n_dma_sems for e in dsems}
            drr = {e: 0 for e in dsems}
            cnt = {e: 0 for e in ENGS}
            sig = [None] * n
            vc = [None] * n
            engvc = {e: {} for e in ENGS}
            dknown = {e: set() for e in ENGS}
            plan = {e: [] for e in ENGS}
            out_dmas = []
            for op in ops:
                e = op.eng
                my = engvc[e]
                waits = []
                for d in sorted(op.deps):
                    dop = ops[d]
                    if dop.dma:
                        if d in dknown[e]:
                            continue
                        dknown[e].add(d)
                        waits.append(sig[d])
                    else:
                        if dop.eng == "pe" and e == "pe":
                            continue
                        c = sig[d][1]
                        if my.get(dop.eng, 0) >= c:
                            continue
                        waits.append((csem[dop.eng], c))
                    for k, v in vc[d].items():
                        if my.get(k, 0) < v:
                            my[k] = v
                if op.dma:
                    k = drr[e]
                    drr[e] = (k + 1) % n_dma_sems
                    s = dsems[e][k]
                    if duse[e][k] > 0:
                        waits.append((s, 16 * duse[e][k]))
                    duse[e][k] += 1
                    sig[op.i] = (s, 16 * duse[e][k])
                    vc[op.i] = dict(my)
                    plan[e].append((waits, op, (s, 16)))
                    if op.out:
                        out_dmas.append(sig[op.i])
                else:
                    if needs[op.i]:
                        cnt[e] += 1
                        sig[op.i] = (csem[e], cnt[e])
                        v = dict(my)
                        v[e] = cnt[e]
                        vc[op.i] = v
                        plan[e].append((waits, op, (csem[e], 1)))
                    else:
                        plan[e].append((waits, op, None))
            self.stats = {e: len(plan[e]) for e in ENGS}

            def run(eng, e):
                for waits, op, s in plan[e]:
                    for sem, val in waits:
                        eng.wait_ge(sem, val)
                    ins = op.fn(eng)
                    if s is not None:
                        ins.then_inc(s[0], s[1])
                if e == "sp":
                    for sem, val in out_dmas:
                        eng.wait_ge(sem, val)

            with nc.Block() as block:
                @block.tensor
                def _(eng):
                    run(eng, "pe")

                @block.scalar
                def _(eng):
                    run(eng, "act")

                @block.vector
                def _(eng):
                    run(eng, "dve")

                @block.gpsimd
                def _(eng):
                    run(eng, "pool")

                @block.sync
                def _(eng):
                    run(eng, "sp")


def tk(name, lo, hi):
    return ["%s.%d" % (name, i) for i in range(lo, hi)]


def build(stop=None):
    nc = bass.Bass("TRN2", target_bir_lowering=False)
    P = Prog()

    def din(name, shape):
        return nc.dram_tensor(name, list(shape), F32, kind="ExternalInput").ap()

    x_d = din("x", [T, D])
    mem_d = din("mem", [256, D])
    w_in_d = din("w_in", [D, 4096])
    w_sw_d = din("w_sw", [D, 1024])
    w_mix_d = din("w_mix", [D, D])
    wq_d = din("xa_wq", [D, D])
    wk_d = din("xa_wk", [D, D])
    wv_d = din("xa_wv", [D, D])
    wo_d = din("xa_wo", [D, D])
    wr_d = din("w_router", [D, 16])
    if stop is None or stop >= 6:
        wg_d = din("w_gate", [16, 16, 128, 1024])
        wu_d = din("w_up", [16, 16, 128, 1024])
        wd_d = din("w_down", [16, 2048, D])
    lng_d = din("ln_g", [4, D])
    lnb_d = din("ln_b", [4, D])
    lb_d = din("hg_lb", [16, 128])
    hgn_d = din("hg_norm_g", [4, 128])
    sub_d = din("da_subln_g", [1, 128])
    lam_d = din("da_lam", [4, 64])
    c_ident = din("c_ident", [128, 128])
    c_aI = din("c_aI", [128, 128])
    c_aI4 = din("c_aI4", [128, 2048])
    c_ones = din("c_ones", [128, 128])
    c_triF = din("c_triF", [128, 128])
    c_triB = din("c_triB", [128, 128])
    c_rmask = din("c_rmask", [128, 512])
    c_ropeC = din("c_ropeC", [128, T])
    c_ropeS = din("c_ropeS", [128, T])
    c_iotaf = din("c_iotaf", [128, 256])
    out_d = nc.dram_tensor("out", [T, D], F32, kind="ExternalOutput").ap()
    dbg = stop is not None
    if dbg:
        dbgA_d = nc.dram_tensor("dbgA", [128, 16384], F32, kind="ExternalOutput").ap()
        dbgB_d = nc.dram_tensor("dbgB", [128, 16384], F32, kind="ExternalOutput").ap()

    st = contextlib.ExitStack()
    with st:
        def sb(name, shape, dt=F32):
            return st.enter_context(nc.sbuf_tensor(name, list(shape), dt))

        RA = sb("RA", [128, 16384], F32R)
        RB = sb("RB", [128, 16384], F32R)
        SR = sb("SR", [128, 12288], F32R)
        FA = sb("FA", [128, 6400], F32)

        def Rb(off, n):
            return SR[:, off:off + n]

        def Fb(off, n):
            return FA[:, off:off + n]
        smallp = sb("smallp", [128, 768], F32)
        ident = sb("ident", [128, 128])
        aI = sb("aI", [128, 128])
        onesR = sb("onesR", [128, 128], F32R)
        gB = FA[:, 2048:3072]
        bB = FA[:, 3072:4096]
        xt = [FA[:, i * 1024:(i + 1) * 1024] for i in range(2)]
        stt = sb("stt", [128, 32])
        cols = sb("cols", [128, 64])
        ps = [st.enter_context(nc.psum_tensor("ps%d" % i, [128, 512], F32)) for i in range(8)]

        hT = RA[:, :].rearrange("p (j t) -> p j t", t=T)
        catT = RB[:, :].rearrange("p (j t) -> p j t", t=T)

        def dma(q, out, in_, r=(), w=(), outp=False):
            return P.add(q, lambda e: e.dma_start(out=out, in_=in_), r, w, dma=True, out=outp)

        def pe(mms, r, w, skip=False):
            def f(e):
                for (o, l, rh, s, t) in mms:
                    ins = e.matmul(o, l, rh, start=s, stop=t, skip_group_check=skip)
                return ins
            return P.add("pe", f, r, w)

        def petr(trs, r, w):
            def f(e):
                for (o, i_, idn) in trs:
                    ins = e.transpose(o, i_, idn)
                return ins
            return P.add("pe", f, r, w)

        def A(fn, r, w):
            return P.add("act", fn, r, w)

        def V(fn, r, w):
            return P.add("dve", fn, r, w)

        _alt = [0]

        def copy_any(out, in_, r, w):
            _alt[0] ^= 1
            if _alt[0]:
                return A(lambda e: e.copy(out, in_), r, w)
            return V(lambda e: e.tensor_copy(out, in_), r, w)

        if dbg:
            V(lambda e: e.memset(RA[:, :], 0.0), [], tk("hT", 0, 16))
            V(lambda e: e.memset(RB[:, :], 0.0), [], ["cat%d.%d" % (k, q) for k in range(8) for q in range(4)])
        dma("sp", ident[:, :], c_ident, w=["ident"])
        dma("sp", aI[:, :], c_aI, w=["aI"])
        dma("pool", onesR[:, :], c_ones, w=["onesR"])

        def load_ln(k):
            dma("sp", gB, lng_d[k].partition_broadcast(128), w=["gB"])
            dma("sp", bB, lnb_d[k].partition_broadcast(128), w=["bB"])

        def ln_chunk_gen(src_lo, src_hi, rtok, dst, dtok, tmp, ttok, par=0):
            so = 16 * par
            sx = "%d" % par
            s6 = stt[:, so:so + 12].rearrange("p (a b) -> p a b", b=6)
            mean, var, rstd, nmr = (stt[:, so + 12:so + 13], stt[:, so + 13:so + 14], stt[:, so + 14:so + 15], stt[:, so + 15:so + 16])
            V(lambda e: e.bn_stats(s6[:, 0, :], src_lo), rtok, ["st6a" + sx])
            yield
            V(lambda e: e.bn_stats(s6[:, 1, :], src_hi), rtok, ["st6b" + sx])
            yield
            V(lambda e: e.bn_aggr(stt[:, so + 12:so + 14], stt[:, so:so + 12]), ["st6a" + sx, "st6b" + sx], ["mv" + sx])
            yield
            V(lambda e: e.tensor_scalar(rstd, var, LN_EPS, None, ALU.add), ["mv" + sx], ["rstd" + sx])
            yield
            A(lambda e: e.activation(rstd, rstd, AF.Ln), ["rstd" + sx], ["rstd" + sx])
            yield
            A(lambda e: e.activation(rstd, rstd, AF.Exp, scale=-0.5), ["rstd" + sx], ["rstd" + sx])
            yield
            V(lambda e: e.scalar_tensor_tensor(nmr, mean, -1.0, rstd, ALU.mult, ALU.mult), ["mv" + sx, "rstd" + sx], ["nmr" + sx])
            yield
            A(lambda e: e.activation(tmp[:, 0:512], src_lo, AF.Identity, bias=nmr, scale=rstd),
              rtok + ["rstd" + sx, "nmr" + sx], [ttok + "a"] + list(dtok))
            yield
            A(lambda e: e.activation(tmp[:, 512:1024], src_hi, AF.Identity, bias=nmr, scale=rstd),
              rtok + ["rstd" + sx, "nmr" + sx], [ttok + "b"] + list(dtok))
            yield
            V(lambda e: e.tensor_tensor(tmp, tmp, gB, ALU.mult), [ttok + "a", ttok + "b", "gB"], [ttok + "a", ttok + "b"])
            yield
            V(lambda e: e.tensor_tensor(dst, tmp, bB, ALU.add), [ttok + "a", ttok + "b", "bB"], dtok)
            yield

        def ln_chunk(src_lo, src_hi, rtok, dst, dtok, tmp, ttok, key):
            for _ in ln_chunk_gen(src_lo, src_hi, rtok, dst, dtok, tmp, ttok, 0):
                pass

        def interleave(*gens):
            gens = [g for g in gens if g is not None]
            while gens:
                for g in list(gens):
                    try:
                        next(g)
                    except StopIteration:
                        gens.remove(g)

        def tm_to_fm(src, stok, tc, pbanks):
            for b in range(2):
                pb = pbanks[b]
                o3 = ps[pb][:, :].rearrange("p (a c) -> p a c", c=128)
                petr([(o3[:, a, :], src[:, (4 * b + a) * 128:(4 * b + a + 1) * 128], ident[:, :]) for a in range(4)],
                     stok + ["ident"], ["ps.%d" % pb])
                copy_any(hT[:, 4 * b:4 * b + 4, tc * 128:(tc + 1) * 128], o3, ["ps.%d" % pb], ["hT.%d" % tc])

        def dump_and_finish():
            dma("sp", dbgA_d, RA[:, :].bitcast(F32), r=["DUMP"], outp=True)
            dma("sp", dbgB_d, RB[:, :].bitcast(F32), r=["DUMP"], outp=True)

        load_ln(0)
        def ln0_chain(tc):
            par = tc % 2
            xb = xt[par]
            xk = "xt%d" % par
            dma("sp", xb[:, :], x_d[tc * 128:(tc + 1) * 128, :], w=[xk])
            yield
            yield from ln_chunk_gen(xb[:, 0:512], xb[:, 512:1024], [xk], xb[:, :], [xk], xb[:, :], xk, par)
            tm_to_fm(xb, [xk], tc, (6, 7) if par == 0 else (4, 5))
            yield

        for tc in range(0, 16, 2):
            interleave(ln0_chain(tc), ln0_chain(tc + 1))

        lbt = FA[0:16, 4096:4224]
        dma("sp", lbt, lb_d, w=["lbt"])
        petr([(ps[0][:, 0:16], lbt, ident[0:16, 0:16])], ["lbt", "ident"], ["ps.0"])
        V(lambda e: e.tensor_copy(cols[:, 48:64], ps[0][:, 0:16]), ["ps.0"], ["c48"])
        psl = cols[:, 48:64].rearrange("p (a l h) -> p a l h", a=2, l=2)
        c_lb = cols[:, 0:8].rearrange("p (a h) -> p a h", a=2)
        c_oml = cols[:, 8:16].rearrange("p (a h) -> p a h", a=2)
        V(lambda e: e.tensor_tensor(c_lb, psl[:, :, 1, :], psl[:, :, 0, :], ALU.subtract), ["c48"], ["c_lb"])
        A(lambda e: e.activation(cols[:, 0:8], cols[:, 0:8], AF.Exp), ["c_lb"], ["c_lb"])
        V(lambda e: e.tensor_scalar(cols[:, 0:8], cols[:, 0:8], 1.0, None, ALU.add), ["c_lb"], ["c_lb"])
        V(lambda e: e.reciprocal(cols[:, 0:8], cols[:, 0:8]), ["c_lb"], ["c_lb"])
        V(lambda e: e.tensor_scalar(cols[:, 8:16], cols[:, 0:8], -1.0, 1.0, ALU.mult, ALU.add), ["c_lb"], ["c_oml"])
        V(lambda e: e.tensor_scalar(cols[:, 40:48], cols[:, 8:16], 0.5, None, ALU.mult), ["c_oml"], ["c_oml"])
        V(lambda e: e.tensor_tensor(cols[:, 24:32], cols[:, 0:8], cols[:, 40:48], ALU.add), ["c_lb", "c_oml"], ["c_lb"])
        gt = FA[0:4, 4224:4352]
        dma("sp", gt, hgn_d, w=["gt"])
        petr([(ps[1][:, 0:4], gt, ident[0:4, 0:4])], ["gt", "ident"], ["ps.1"])
        V(lambda e: e.tensor_scalar(cols[:, 16:20], ps[1][:, 0:4], math.sqrt(128.0), None, ALU.mult), ["ps.1"], ["c_hgn"])
        sg1 = FA[0:1, 4352:4480]
        dma("sp", sg1, sub_d, w=["sg1"])
        petr([(ps[2][:, 0:1], sg1, ident[0:1, 0:1])], ["sg1", "ident"], ["ps.2"])
        V(lambda e: e.tensor_scalar(cols[:, 20:21], ps[2][:, 0:1], math.sqrt(128.0) * (1.0 - LAMBDA_INIT), None, ALU.mult),
          ["ps.2"], ["c_sub"])
        lmb = FA[:, 4480:4736].rearrange("p (a b) -> p a b", b=64)
        for a in range(4):
            dma("sp", lmb[:, a, :], lam_d[a].partition_broadcast(128), w=["lmb%d" % a])
        V(lambda e: e.tensor_tensor(lmb[:, 0, :], lmb[:, 0, :], lmb[:, 1, :], ALU.mult), ["lmb0", "lmb1"], ["lmb0"])
        V(lambda e: e.tensor_tensor(lmb[:, 2, :], lmb[:, 2, :], lmb[:, 3, :], ALU.mult), ["lmb2", "lmb3"], ["lmb2"])
        V(lambda e: e.reduce_sum(cols[:, 22:23], lmb[:, 0, :], axis=AX.X), ["lmb0"], ["c22"])
        V(lambda e: e.reduce_sum(cols[:, 23:24], lmb[:, 2, :], axis=AX.X), ["lmb2"], ["c23"])
        A(lambda e: e.activation(cols[:, 22:24], cols[:, 22:24], AF.Exp), ["c22", "c23"], ["c22", "c23"])
        V(lambda e: e.tensor_tensor(cols[:, 21:22], cols[:, 23:24], cols[:, 22:23], ALU.subtract), ["c22", "c23"], ["c_nlam"])
        V(lambda e: e.tensor_scalar(cols[:, 21:22], cols[:, 21:22], -LAMBDA_INIT, None, ALU.add), ["c_nlam"], ["c_nlam"])
        P.barrier()

        if stop == 0:
            P.add("pool", lambda e: e.nop(), [], ["DUMP"])
            dump_and_finish()
            P.emit(nc)
            return nc

        wctr = [0]
        ring = {"base": 3584, "n": 7}

        def wslice(w_d, c0):
            k = wctr[0] % ring["n"]
            wctr[0] += 1
            o_ = ring["base"] + k * 1024
            v = SR[:, o_:o_ + 1024].rearrange("p (j c) -> p j c", c=128)
            dma("pool", v, w_d.rearrange("(j p) c -> p j c", p=128)[:, :, c0:c0 + 128], w=["WB.%d" % k])
            return v, "WB.%d" % k

        def projB(wv_, wtok, tb, pb):
            pe([(ps[pb][:, :], wv_[:, j, :], hT[:, j, tb * 512:(tb + 1) * 512], j == 0, j == 7) for j in range(8)],
               [wtok] + tk("hT", tb * 4, tb * 4 + 4), ["ps.%d" % pb])


        triF = Fb(4096, 128)
        triB = Fb(4224, 128)
        rmask = Fb(4352, 512)
        dma("sp", triF, c_triF, w=["triF"])
        dma("sp", triB, c_triB, w=["triB"])
        dma("sp", rmask, c_rmask, w=["rmask"])
        qs, fT, kT, Pc, eA = [Fb(i * 512, 512) for i in range(5)]
        Sf = [Fb(4864, 1152), Fb(2560, 1152)]
        ring["base"], ring["n"] = 7168, 5
        pt = "b0"

        def rbuf(par, off, n=512):
            return Rb(par * 3584 + off, n)

        blocks = [(hh, di, bi, tb) for hh in range(4) for di in range(2)
                  for bi, tb in enumerate([0, 1, 2, 3] if di == 0 else [3, 2, 1, 0])]
        ctxs = {}
        wcur = {}

        def projs(k):
            hh, di, bi, tb = blocks[k]
            if bi == 0:
                wcur[(hh, di)] = (wslice(w_in_d, 0 + hh * 128), wslice(w_in_d, 512 + hh * 128),
                                  wslice(w_in_d, (1536 if di == 0 else 2048) + hh * 128))
            (wq_v, wq_t), (wi_v, wi_t), (wf_v, wf_t) = wcur[(hh, di)]
            projB(wq_v, wq_t, tb, 6)
            projB(wf_v, wf_t, tb, 7)
            projB(wi_v, wi_t, tb, k % 2)

        def stageA1(k):
            hh, di, bi, tb = blocks[k]
            par = k % 2
            rt = "r%d" % par
            lbc = cols[:, 24 + di * 4 + hh:24 + di * 4 + hh + 1]
            omlc = cols[:, 40 + di * 4 + hh:40 + di * 4 + hh + 1]
            qin, kin = rbuf(par, 0), rbuf(par, 512)
            koTM = rbuf(par, 1024).rearrange("p (a c) -> p a c", c=128)
            att = rbuf(par, 1536)
            vTM = rbuf(par, 2048).rearrange("p (a c) -> p a c", c=128)
            SentF = rbuf(par, 2560, 1024).rearrange("p (i c) -> p i c", c=128)
            dec = cols[:, 32:40] if par == 0 else cols[:, 56:64]
            ctxs[k] = dict(qin=qin, kin=kin, koTM=koTM, att=att, vTM=vTM, SentF=SentF, dec=dec)
            A(lambda e: e.activation(qs, ps[6][:, :], AF.Silu), ["ps.6"], [pt + "qs"])
            yield
            A(lambda e: e.activation(fT, ps[7][:, :], AF.Tanh, scale=0.5), ["ps.7"], [pt + "fT"])
            yield
            V(lambda e: e.tensor_scalar(fT, fT, omlc, lbc, ALU.mult, ALU.add), [pt + "fT", "c_lb", "c_oml"], [pt + "fT"])
            yield
            V(lambda e: e.tensor_scalar(kT, fT, -1.0, 1.0, ALU.mult, ALU.add), [pt + "fT"], [pt + "kT"])
            yield
            A(lambda e: e.activation(fT, fT, AF.Ln), [pt + "fT"], [pt + "fT"])
            yield
            V(lambda e: e.tensor_tensor_scan(Pc, rmask, fT, 0.0, ALU.mult, ALU.add), [pt + "fT", "rmask"], [pt + "Pc"])
            yield
            P3 = Pc.rearrange("p (n c) -> p n c", c=64)
            f3 = fT.rearrange("p (n c) -> p n c", c=64)
            tot = P3[:, :, 63:64]
            totb = tot.broadcast_to([128, 8, 64])
            A(lambda e: e.activation(dec.rearrange("p (n o) -> p n o", o=1), tot, AF.Exp), [pt + "Pc"], [rt + "dec"])
            yield
            eA3 = eA.rearrange("p (n c) -> p n c", c=64)
            if di == 0:
                A(lambda e: e.activation(eA, Pc, AF.Exp), [pt + "Pc"], [pt + "eA"])
                yield
                V(lambda e: e.tensor_tensor(qin, qs, eA, ALU.mult), [pt + "qs", pt + "eA"], [rt + "qin"])
                yield
                A(lambda e: e.activation(eA, Pc, AF.Exp, scale=-1.0), [pt + "Pc", rt + "qin"], [pt + "eA"])
                yield
                V(lambda e: e.tensor_tensor(kin, kT, eA, ALU.mult), [pt + "kT", pt + "eA"], [rt + "kin"])
                yield
                V(lambda e: e.tensor_tensor(f3, totb, P3, ALU.subtract), [pt + "Pc"], [pt + "fT"])
                yield
            else:
                V(lambda e: e.tensor_tensor(fT, Pc, fT, ALU.subtract), [pt + "Pc", pt + "fT"], [pt + "fT"])
                yield
                V(lambda e: e.tensor_tensor(eA3, totb, f3, ALU.subtract), [pt + "Pc", pt + "fT"], [pt + "eA"])
                yield
                A(lambda e: e.activation(Pc, eA, AF.Exp), [pt + "eA", rt + "dec"], [pt + "Pc"])
                yield
                V(lambda e: e.tensor_tensor(qin, qs, Pc, ALU.mult), [pt + "qs", pt + "Pc"], [rt + "qin"])
                yield
                A(lambda e: e.activation(Pc, eA, AF.Exp, scale=-1.0), [pt + "eA", rt + "qin"], [pt + "Pc"])
                yield
                V(lambda e: e.tensor_tensor(kin, kT, Pc, ALU.mult), [pt + "kT", pt + "Pc"], [rt + "kin"])
                yield
            if k + 1 < len(blocks):
                projs(k + 1)
                yield
            A(lambda e: e.activation(fT, fT, AF.Exp), [pt + "fT"], [pt + "fT"])
            yield
            V(lambda e: e.tensor_tensor(kT, kT, fT, ALU.mult), [pt + "kT", pt + "fT", rt + "kin"], [pt + "kT"])
            yield
            A(lambda e: e.copy(eA, ps[k % 2][:, :]), ["ps.%d" % (k % 2), rt + "kin", rt + "qin"], [pt + "eA"])
            yield
        def stageA2(k):
            par = k % 2
            rt = "r%d" % par
            koTM, vTM = ctxs[k]["koTM"], ctxs[k]["vTM"]
            o3 = ps[5][:, :].rearrange("p (a c) -> p a c", c=128)
            petr([(o3[:, a, :], kT[:, a * 128:(a + 1) * 128], ident[:, :]) for a in range(4)], [pt + "kT", "ident"], ["ps.5"])
            A(lambda e: e.copy(koTM, o3), ["ps.5"], [rt + "koTM"])
            o3b = ps[2][:, :].rearrange("p (a c) -> p a c", c=128)
            petr([(o3b[:, a, :], eA[:, a * 128:(a + 1) * 128], ident[:, :]) for a in range(4)], [pt + "eA", "ident"], ["ps.2"])
            V(lambda e: e.tensor_copy(vTM, o3b), ["ps.2"], [rt + "vTM"])

        def stageB(k):
            hh, di, bi, tb = blocks[k]
            par = k % 2
            rt = "r%d" % par
            c = ctxs.pop(k)
            qin, kin, koTM, att, vTM, SentF, dec = c["qin"], c["kin"], c["koTM"], c["att"], c["vTM"], c["SentF"], c["dec"]
            tri = triF if di == 0 else triB
            tritok = "triF" if di == 0 else "triB"
            for sc in range(4):
                pe([(ps[5][:, sc * 128:(sc + 1) * 128], kin[:, sc * 128:(sc + 1) * 128], qin[:, sc * 128:(sc + 1) * 128], True, True)],
                   [rt + "kin", rt + "qin"], ["ps.5"])
                yield
            a3 = att.rearrange("p (a c) -> p a c", c=128)
            p53 = ps[5][:, :].rearrange("p (a c) -> p a c", c=128)
            trib = tri[:, None, :].broadcast_to([128, 4, 128])
            V(lambda e: e.tensor_tensor(a3, p53, trib, ALU.mult), ["ps.5", tritok], [rt + "att"])
            yield
            for n in range(8):
                a, r0 = n // 2, (n % 2) * 64
                pbk = 3 + n % 2
                pe([(ps[pbk][:, a * 128:(a + 1) * 128], koTM[r0:r0 + 64, a, :], vTM[r0:r0 + 64, a, :], True, True)],
                   [rt + "koTM", rt + "vTM"], ["ps.%d" % pbk])
                yield
            S = Sf[par].rearrange("p (i c) -> p i c", c=128)
            Sprev = Sf[1 - par].rearrange("p (i c) -> p i c", c=128)
            corder = list(range(8)) if di == 0 else list(range(7, -1, -1))
            if bi == 0:
                V(lambda e: e.memset(S[:, 0, :], 0.0), [], ["S%d_0" % par])
                yield
            else:
                V(lambda e: e.tensor_copy(S[:, 0, :], Sprev[:, 8, :]), ["S%d_8" % (1 - par)], ["S%d_0" % par])
                yield
            for i, n in enumerate(corder):
                pbk = 3 + n % 2
                dl = ps[pbk][:, (n // 2) * 128:(n // 2 + 1) * 128]
                V(lambda e, i=i, n=n, dl=dl: e.scalar_tensor_tensor(S[:, i + 1, :], S[:, i, :], dec[:, n:n + 1], dl, ALU.mult, ALU.add),
                  ["S%d_%d" % (par, i), rt + "dec", "ps.%d" % pbk], ["S%d_%d" % (par, i + 1)])
                yield
            A(lambda e: e.copy(SentF, S[:, 0:8, :]), ["S%d_%d" % (par, i) for i in range(8)], [rt + "SentF"])
            yield
            mms = []
            for sc in range(4):
                mms.append((ps[2][:, sc * 128:(sc + 1) * 128], vTM[:, sc, :], a3[:, sc, :], True, False))
                for cch in range(2):
                    n = sc * 2 + cch
                    i = corder.index(n)
                    mms.append((ps[2][:, n * 64:(n + 1) * 64], SentF[:, i, :], qin[:, n * 64:(n + 1) * 64], False, cch == 1))
            pe(mms, [rt + "vTM", rt + "att", rt + "SentF", rt + "qin"], ["ps.2"])
            yield
            dstc = catT[:, hh, tb * 512:(tb + 1) * 512]
            ctoks = tk("cat%d" % hh, tb, tb + 1)
            if di == 0:
                A(lambda e: e.copy(dstc, ps[2][:, :]), ["ps.2"], ctoks)
                yield
            else:
                V(lambda e: e.tensor_tensor(dstc, dstc.bitcast(F32), ps[2][:, :], ALU.add), ["ps.2"] + ctoks, ctoks)
                yield

        def epilogue(hh, par):
            rt = "r%d" % par
            wg_v, wg_t = wslice(w_in_d, 1024 + hh * 128)
            sgbs = [qs, fT, Pc, eA]
            stoks = [pt + "qs", pt + "fT", pt + "Pc", pt + "eA"]
            sqs = [rbuf(par, 2048), rbuf(par, 1536)]
            sqtk = [rt + "vTM", rt + "att"]
            t1 = kT
            gcol = cols[:, 16 + hh:17 + hh]
            for tb in range(4):
                pb = 2 if tb % 2 == 0 else 5
                projB(wg_v, wg_t, tb, pb)
                A(lambda e, tb=tb, pb=pb: e.activation(sgbs[tb], ps[pb][:, :], AF.Silu), ["ps.%d" % pb], [stoks[tb]])
            for tb in range(4):
                oc = catT[:, hh, tb * 512:(tb + 1) * 512]
                ctoks = tk("cat%d" % hh, tb, tb + 1)
                sq = sqs[tb % 2]
                pz = 3 + tb % 2
                A(lambda e, oc=oc, sq=sq: e.activation(sq, oc.bitcast(F32), AF.Square), ctoks, [sqtk[tb % 2]])
                pe([(ps[pz][:, :], onesR[:, :], sq, True, True)], [sqtk[tb % 2], "onesR"], ["ps.%d" % pz])
                V(lambda e, pz=pz: e.tensor_scalar(t1, ps[pz][:, :], 128.0 * RMS_EPS, None, ALU.add), ["ps.%d" % pz], [pt + "kT"])
                A(lambda e: e.activation(t1, t1, AF.Ln), [pt + "kT"], [pt + "kT"])
                A(lambda e: e.activation(t1, t1, AF.Exp, scale=-0.5), [pt + "kT"], [pt + "kT"])
                V(lambda e, tb=tb: e.scalar_tensor_tensor(t1, t1, gcol, sgbs[tb], ALU.mult, ALU.mult), [pt + "kT", stoks[tb], "c_hgn"], [pt + "kT"])
                V(lambda e, oc=oc: e.tensor_tensor(oc, oc.bitcast(F32), t1, ALU.mult), [pt + "kT"] + ctoks, ctoks)

        def run_interleaved(*gens):
            gens = [g for g in gens if g is not None]
            while gens:
                for g in list(gens):
                    try:
                        next(g)
                    except StopIteration:
                        gens.remove(g)

        projs(0)
        run_interleaved(stageA1(0))
        stageA2(0)
        for k in range(len(blocks)):
            run_interleaved(stageA1(k + 1) if k + 1 < len(blocks) else None, stageB(k))
            if k + 1 < len(blocks):
                stageA2(k + 1)
            hh, di, bi, tb = blocks[k]
            if di == 1 and bi == 3:
                epilogue(hh, k % 2)
        P.barrier()
        if stop == 1:
            P.add("pool", lambda e: e.nop(), [], ["DUMP"])
            dump_and_finish()
            P.emit(nc)
            return nc

        ring["base"], ring["n"] = 8704, 3
        wctr[0] = 0
        ropeC = Fb(0, 2048)
        ropeS = Fb(2048, 2048)
        tmpq = Fb(5120, 512)
        tmpvv = Fb(5632, 512)
        sqb = Rb(8192, 512)
        onesF = Fb(6144, 128)
        dma("sp", onesF, c_ones, w=["onesF"])
        dma("sp", ropeC, c_ropeC, w=["ropeC"])
        dma("sp", ropeS, c_ropeS, w=["ropeS"])
        qT = Rb(0, 2048)
        kTt = Rb(2048, 2048)
        VTM = Rb(4096, 2048).rearrange("p (a c) -> p a c", c=128)
        Er = [Rb(6144 + i * 512, 512) for i in range(4)]
        for hh in range(4):
            pstep = 0
            for which, dst, base, swb in (("q", qT, 2560, 0), ("k", kTt, 3072, 512)):
                w1, t1_ = wslice(w_in_d, base + hh * 128)
                w2, t2_ = wslice(w_sw_d, swb + hh * 128)
                for tb in range(4):
                    pa = 2 * (pstep % 2)
                    pb_ = pa + 1
                    tmpb = [tmpq, tmpvv][pstep % 2]
                    ttk = ["tmpq", "tmpvv"][pstep % 2]
                    pstep += 1
                    projB(w1, t1_, tb, pa)
                    projB(w2, t2_, tb, pb_)
                    d_ = dst[:, tb * 512:(tb + 1) * 512]
                    V(lambda e, tmpb=tmpb, tb=tb, pb_=pb_: e.tensor_tensor(tmpb, ps[pb_][:, :], ropeS[:, tb * 512:(tb + 1) * 512], ALU.mult),
                      ["ps.%d" % pb_, "ropeS"], [ttk])
                    V(lambda e, d_=d_, tb=tb, pa=pa: e.tensor_tensor(d_, ps[pa][:, :], ropeC[:, tb * 512:(tb + 1) * 512], ALU.mult),
                      ["ps.%d" % pa, "ropeC"], ["%sT.%d" % (which, tb)])
                    V(lambda e, d_=d_, tmpb=tmpb: e.tensor_tensor(d_, d_.bitcast(F32), tmpb, ALU.add),
                      [ttk, "%sT.%d" % (which, tb)], ["%sT.%d" % (which, tb)])
            wv_v, wv_t = wslice(w_in_d, 3584 + hh * 128)
            for tb in range(4):
                pv = 4 + tb % 2
                pt_ = 6 + tb % 2
                tmpb = [Fb(4096, 512), Fb(4608, 512)][tb % 2]
                ttk = ["r0", "r1"][tb % 2]
                projB(wv_v, wv_t, tb, pv)
                A(lambda e, tmpb=tmpb, pv=pv: e.copy(tmpb, ps[pv][:, :]), ["ps.%d" % pv], [ttk])
                o3 = ps[pt_][:, :].rearrange("p (a c) -> p a c", c=128)
                petr([(o3[:, a, :], tmpb[:, a * 128:(a + 1) * 128], ident[:, :]) for a in range(4)], [ttk, "ident"], ["ps.%d" % pt_])
                V(lambda e, tb=tb, o3=o3: e.tensor_copy(VTM[:, tb * 4:tb * 4 + 4, :], o3), ["ps.%d" % pt_], ["VTM.%d" % tb])
            tails = []
            for qb in range(4):
                steps = [(j, kp) for j in range(2) for kp in range(8)]
                stb = [(0, 1), (4, 5)]
                Zacc = [tmpq, tmpvv]
                ztok = ["tmpq", "tmpvv"]

                def st_mm(i):
                    j, kp = steps[i]
                    r0 = j * 64
                    bb = stb[i % 2]
                    pe([(ps[bb[h2]][:, :], kTt[r0:r0 + 64, (2 * kp + h2) * 128:(2 * kp + h2 + 1) * 128], qT[r0:r0 + 64, qb * 512:(qb + 1) * 512], True, True)
                        for h2 in range(2)], ["kT.%d" % (kp // 2), "qT.%d" % qb], ["ps.%d" % bb[0], "ps.%d" % bb[1]])

                st_mm(0)
                for i, (j, kp) in enumerate(steps):
                    if i + 1 < len(steps):
                        st_mm(i + 1)
                    bb = stb[i % 2]
                    ebs = [Er[(i % 2) * 2 + h2] for h2 in range(2)]
                    etk = ["E%d" % ((i % 2) * 2 + h2) for h2 in range(2)]
                    for h2 in range(2):
                        A(lambda e, Eb=ebs[h2], pb=bb[h2]: e.activation(Eb, ps[pb][:, :], AF.Exp, scale=0.125), ["ps.%d" % bb[h2]], [etk[h2]])
                    pe([(ps[2 + j][:, :], VTM[:, 2 * kp + h2, :], ebs[h2], (2 * kp + h2) == 0, (2 * kp + h2) == 15) for h2 in range(2)],
                       ["VTM.%d" % (kp // 2)] + etk, ["ps.%d" % (2 + j)])
                    for h2 in range(2):
                        if 2 * kp + h2 == 0:
                            V(lambda e, Z=Zacc[j], Eb=ebs[h2]: e.tensor_copy(Z, Eb.bitcast(F32)), [etk[h2]], [ztok[j]])
                        else:
                            V(lambda e, Z=Zacc[j], Eb=ebs[h2]: e.tensor_tensor(Z, Z, Eb.bitcast(F32), ALU.add), [etk[h2], ztok[j]], [ztok[j]])
                    if i in (1, 2) and tails:
                        tails.pop(0)()
                for j in range(2):
                    pe([(ps[6 + j][:, :], onesF, Zacc[j], True, True)], ["onesF", ztok[j]], ["ps.%d" % (6 + j)])
                r_0 = Fb(4096, 512)
                r_1 = Fb(4608, 512)
                A(lambda e, r_0=r_0: e.activation(r_0, ps[6][:, :], AF.Ln), ["ps.6"], ["r0"])
                A(lambda e, r_1=r_1: e.activation(r_1, ps[7][:, :], AF.Ln), ["ps.7"], ["r1"])
                A(lambda e, r_0=r_0: e.activation(r_0, r_0, AF.Exp, scale=-1.0), ["r0"], ["r0"])
                A(lambda e, r_1=r_1: e.activation(r_1, r_1, AF.Exp, scale=-1.0), ["r1"], ["r1"])
                V(lambda e, r_0=r_0: e.tensor_tensor(r_0, r_0, ps[2][:, :], ALU.mult), ["r0", "ps.2"], ["r0"])
                V(lambda e, r_1=r_1: e.tensor_tensor(r_1, r_1, ps[3][:, :], ALU.mult), ["r1", "ps.3"], ["r1"])
                def tail1(r_0=r_0, r_1=r_1):
                    V(lambda e: e.scalar_tensor_tensor(r_0, r_1, cols[:, 21:22], r_0, ALU.mult, ALU.add), ["r0", "r1", "c_nlam"], ["r0"])
                    A(lambda e: e.activation(sqb, r_0, AF.Square), ["r0"], ["sqb"])
                    pe([(ps[6][:, :], onesR[:, :], sqb, True, True)], ["sqb", "onesR"], ["ps.6"])

                def tail2(r_0=r_0, r_1=r_1, qb=qb, hh=hh):
                    V(lambda e: e.tensor_scalar(r_1, ps[6][:, :], 128.0 * RMS_EPS, None, ALU.add), ["ps.6", "r1"], ["r1"])
                    A(lambda e: e.activation(r_1, r_1, AF.Ln), ["r1"], ["r1"])
                    A(lambda e: e.activation(r_1, r_1, AF.Exp, scale=-0.5), ["r1"], ["r1"])
                    oc = catT[:, 4 + hh, qb * 512:(qb + 1) * 512]
                    V(lambda e: e.scalar_tensor_tensor(oc, r_0, cols[:, 20:21], r_1, ALU.mult, ALU.mult),
                      ["r0", "r1", "c_sub"], tk("cat%d" % (4 + hh), qb, qb + 1))

                tails += [tail1, tail2]
            while tails:
                tails.pop(0)()
        P.barrier()
        if stop == 2:
            P.add("pool", lambda e: e.nop(), [], ["DUMP"])
            dump_and_finish()
            P.emit(nc)
            return nc

        def load_wfull(w_d):
            v = SR[:, 0:8192].rearrange("p (j c) -> p j c", c=1024)
            for q4 in range(4):
                dma("pool", v[:, 2 * q4:2 * q4 + 2, :], w_d.rearrange("(j p) c -> p j c", p=128)[:, 2 * q4:2 * q4 + 2, :],
                    w=["WB.%d" % (2 * q4), "WB.%d" % (2 * q4 + 1)])
            return v

        def proj_ln(wfull, tm_dst=None):
            aI4 = SR[:, 8192:10240].rearrange("p (j c) -> p j c", c=512)
            dma("pool", SR[:, 8192:10240], c_aI4, w=["aI4"])

            def emit_mm(tc):
                p0 = 2 * (tc % 2)
                for b in range(2):
                    mms = []
                    for jj in range(4):
                        mms.append((ps[p0 + b][:, :], hT[:, 4 * b + jj, tc * 128:(tc + 1) * 128], aI4[:, jj, :], jj == 0, False))
                    for k in range(8):
                        mms.append((ps[p0 + b][:, :], catT[:, k, tc * 128:(tc + 1) * 128], wfull[:, k, b * 512:(b + 1) * 512], False, k == 7))
                    pe(mms, ["hT.%d" % tc, "aI4"] + ["cat%d.%d" % (k, tc // 4) for k in range(8)] + tk("WB", 0, 8), ["ps.%d" % (p0 + b)])

            emit_mm(0)
            for tc in range(16):
                p0 = 2 * (tc % 2)
                if tc + 1 < 16:
                    emit_mm(tc + 1)
                xb = xt[tc % 2]
                ln_chunk(ps[p0][:, :], ps[p0 + 1][:, :], ["ps.%d" % p0, "ps.%d" % (p0 + 1)], xb[:, :], ["xt%d" % (tc % 2)], xb[:, :], "xt%d" % (tc % 2), "ln")
                tm_to_fm(xb, ["xt%d" % (tc % 2)], tc, (4 + p0, 5 + p0))

        wfull = load_wfull(w_mix_d)
        load_ln(1)
        proj_ln(wfull)
        P.barrier()
        if stop == 3:
            P.add("pool", lambda e: e.nop(), [], ["DUMP"])
            dump_and_finish()
            P.emit(nc)
            return nc

        ring["base"], ring["n"] = 9216, 3
        wctr[0] = 0
        memTM = Fb(0, 2048).rearrange("p (a c) -> p a c", c=1024)
        memT = Rb(0, 2048).rearrange("p (j m) -> p j m", m=256)
        KmT = Rb(2048, 2048).rearrange("p (j m) -> p j m", m=256)
        Vm = Rb(4096, 2048).rearrange("p (a c) -> p a c", c=1024)
        qxT = Rb(6144, 2048).rearrange("p (a t) -> p a t", t=1024)
        Ex = [Rb(8192 + i * 512, 512) for i in range(2)]
        tmpv4 = Fb(4608, 512)
        for a in range(2):
            dma("sp", memTM[:, a, :], mem_d[a * 128:(a + 1) * 128, :], w=["memTM%d" % a])
        for a in range(2):
            for b in range(2):
                o3 = ps[6 + b][:, :].rearrange("p (q c) -> p q c", c=128)
                petr([(o3[:, q, :], memTM[:, a, (4 * b + q) * 128:(4 * b + q + 1) * 128], ident[:, :]) for q in range(4)],
                     ["memTM%d" % a, "ident"], ["ps.%d" % (6 + b)])
                copy_any(memT[:, 4 * b:4 * b + 4, a * 128:(a + 1) * 128], o3, ["ps.%d" % (6 + b)], ["memT.%d.%d" % (a, b)])
        memtoks = ["memT.%d.%d" % (a_, b_) for a_ in range(2) for b_ in range(2)]
        for ec in range(8):
            wv_, wt_ = wslice(wk_d, ec * 128)
            pk = 6 + ec % 2
            pe([(ps[pk][:, 0:256], wv_[:, j, :], memT[:, j, :], j == 0, j == 7) for j in range(8)], [wt_] + memtoks, ["ps.%d" % pk])
            copy_any(KmT[:, ec, :], ps[pk][:, 0:256], ["ps.%d" % pk], ["KmT.%d" % ec])
        tmpvs = [tmpv4, Fb(5120, 512)]
        for ec in range(8):
            wv_, wt_ = wslice(wv_d, ec * 128)
            pv_, pt_ = (6, 7) if ec % 2 == 0 else (4, 5)
            tmpv = tmpvs[ec % 2]
            tvk = "tmpv4_%d" % (ec % 2)
            pe([(ps[pv_][:, 0:256], wv_[:, j, :], memT[:, j, :], j == 0, j == 7) for j in range(8)], [wt_] + memtoks, ["ps.%d" % pv_])
            A(lambda e, tmpv=tmpv, pv_=pv_: e.copy(tmpv[:, 0:256], ps[pv_][:, 0:256]), ["ps.%d" % pv_], [tvk])
            petr([(ps[pt_][:, a * 128:(a + 1) * 128], tmpv[:, a * 128:(a + 1) * 128], ident[:, :]) for a in range(2)], [tvk, "ident"], ["ps.%d" % pt_])
            V(lambda e, ec=ec, pt_=pt_: e.tensor_copy(Vm[:, :, ec * 128:(ec + 1) * 128], ps[pt_][:, 0:256].rearrange("p (a c) -> p a c", c=128)),
              ["ps.%d" % pt_], ["Vm.%d" % ec])
        xoT = catT
        qx = [qxT, Rb(0, 2048).rearrange("p (a t) -> p a t", t=1024)]

        def qproj_gen(m):
            hx, half = divmod(m, 2)
            q = qx[m % 2]
            extra = memtoks if m % 2 == 1 else []
            for ec in range(2):
                wv_, wt_ = wslice(wq_d, hx * 256 + ec * 128)
                for tbl in range(2):
                    tb = half * 2 + tbl
                    pb = 6 + tbl
                    projB(wv_, wt_, tb, pb)
                    yield
                    copy_any(q[:, ec, tbl * 512:(tbl + 1) * 512], ps[pb][:, :], ["ps.%d" % pb], ["qx%d_%d.%d" % (m % 2, ec, tbl)] + extra)
                    yield

        def attn_gen(m):
            hx, half = divmod(m, 2)
            q = qx[m % 2]
            qk = "qx%d_" % (m % 2)
            for tbl in range(2):
                tb = half * 2 + tbl
                for mc in range(2):
                    pe([(ps[mc][:, :], KmT[:, hx * 2 + ec, mc * 128:(mc + 1) * 128], q[:, ec, tbl * 512:(tbl + 1) * 512], ec == 0, ec == 1)
                        for ec in range(2)], ["KmT.%d" % (hx * 2), "KmT.%d" % (hx * 2 + 1), qk + "0.%d" % tbl, qk + "1.%d" % tbl], ["ps.%d" % mc])
                    yield
                    A(lambda e, mc=mc: e.activation(Ex[mc], ps[mc][:, :], AF.Exp, scale=1.0 / 16.0), ["ps.%d" % mc], ["Ex%d" % mc])
                    yield
                for ec2 in range(2):
                    pe([(ps[2 + ec2][:, :], Vm[:, mc, hx * 256 + ec2 * 128:hx * 256 + (ec2 + 1) * 128], Ex[mc], mc == 0, mc == 1)
                        for mc in range(2)], ["Vm.%d" % (hx * 2 + ec2), "Ex0", "Ex1"], ["ps.%d" % (2 + ec2)])
                    yield
                pe([(ps[4][:, :], onesR[:, :], Ex[mc], mc == 0, mc == 1) for mc in range(2)], ["onesR", "Ex0", "Ex1"], ["ps.4"])
                yield
                rz = Fb(4096, 512)
                A(lambda e, rz=rz: e.activation(rz, ps[4][:, :], AF.Ln), ["ps.4"], ["rz"])
                yield
                A(lambda e, rz=rz: e.activation(rz, rz, AF.Exp, scale=-1.0), ["rz"], ["rz"])
                yield
                for ec2 in range(2):
                    oc = xoT[:, hx * 2 + ec2, tb * 512:(tb + 1) * 512]
                    V(lambda e, oc=oc, rz=rz, ec2=ec2: e.tensor_tensor(oc, ps[2 + ec2][:, :], rz, ALU.mult),
                      ["ps.%d" % (2 + ec2), "rz"], tk("cat%d" % (hx * 2 + ec2), tb, tb + 1))
                    yield

        interleave(qproj_gen(0))
        for m in range(8):
            interleave(qproj_gen(m + 1) if m + 1 < 8 else None, attn_gen(m))
        P.barrier()
        wfull = load_wfull(wo_d)
        load_ln(2)
        proj_ln(wfull)
        P.barrier()
        if stop == 4:
            P.add("pool", lambda e: e.nop(), [], ["DUMP"])
            dump_and_finish()
            P.emit(nc)
            return nc

        h2TM = RB[:, :].rearrange("p (a c) -> p a c", c=1024)
        wr = Fb(4096, 128).rearrange("p (j c) -> p j c", c=16)
        dma("sp", wr, wr_d.rearrange("(j p) c -> p j c", p=128), w=["wr"])
        affT = FA[0:16, 0:2048]
        mskT = FA[0:16, 2048:4096]
        posT = FA[0:16, 4224:6272]
        mgT = affT
        ones16T = FA[0:16, 6272:6288]
        bs = FA[0:16, 6288:6304]
        iotaf = smallp[:, 0:256]
        posTM = smallp[:, 256:512]
        mgTM = smallp[:, 512:768]
        dma("sp", iotaf, c_iotaf, w=["iotaf"])
        V(lambda e: e.memset(ones16T, 1.0), [], ["ones16T"])
        for tb in range(4):
            pe([(ps[tb][0:16, :], wr[:, j, :], hT[:, j, tb * 512:(tb + 1) * 512].bitcast(F32), j == 0, j == 7) for j in range(8)],
               ["wr"] + tk("hT", tb * 4, tb * 4 + 4), ["ps.%d" % tb])
            A(lambda e, tb=tb: e.activation(affT[:, tb * 512:(tb + 1) * 512], ps[tb][0:16, :], AF.Exp), ["ps.%d" % tb], ["affT"])
        for tb in range(4):
            pe([(ps[4][0:16, :], ones16T[:, 0:16], affT[:, tb * 512:(tb + 1) * 512], True, True)], ["affT", "ones16T"], ["ps.4"])
            A(lambda e, tb=tb: e.activation(mskT[:, tb * 512:(tb + 1) * 512], ps[4][0:16, :], AF.Ln), ["ps.4"], ["mskT"])
            A(lambda e, tb=tb: e.activation(mskT[:, tb * 512:(tb + 1) * 512], mskT[:, tb * 512:(tb + 1) * 512], AF.Exp, scale=-1.0), ["mskT"], ["mskT"])
        V(lambda e: e.tensor_tensor(affT, affT, mskT, ALU.mult), ["affT", "mskT"], ["affT"])
        V(lambda e: e.memset(bs[:, 0:1], 0.0), [], ["bs"])
        for it in range(NITER):
            wi = 2.0 ** -(it + 1)
            V(lambda e, wi=wi: e.tensor_scalar(bs[:, 2:3], bs[:, 0:1], wi, None, ALU.add), ["bs"], ["bs"])
            V(lambda e: e.tensor_scalar(mskT, affT, bs[:, 2:3], 0.0, ALU.is_ge, ALU.add, accum_out=bs[:, 3:4]), ["bs", "affT"], ["mskT", "bs"])
            V(lambda e, wi=wi: e.tensor_scalar(bs[:, 4:5], bs[:, 3:4], float(CAP), wi, ALU.is_ge, ALU.mult), ["bs"], ["bs"])
            V(lambda e: e.tensor_tensor(bs[:, 0:1], bs[:, 0:1], bs[:, 4:5], ALU.add), ["bs"], ["bs"])
        V(lambda e: e.tensor_scalar(mskT, affT, bs[:, 0:1], None, ALU.is_ge), ["bs", "affT"], ["mskT"])
        V(lambda e: e.tensor_tensor_scan(posT, ones16T[:, 0:1].broadcast_to([16, T]), mskT, 0.0, ALU.mult, ALU.add), ["mskT", "ones16T"], ["posT"])
        V(lambda e: e.tensor_tensor(posT, posT, mskT, ALU.mult), ["posT", "mskT"], ["posT"])
        V(lambda e: e.tensor_scalar(posT, posT, -1.0, None, ALU.add), ["posT"], ["posT"])
        V(lambda e: e.tensor_tensor(mgT, mskT, affT, ALU.mult), ["mskT", "affT"], ["affT", "mgT"])
        for tc in range(16):
            for b in range(2):
                o3 = ps[6 + b][:, :].rearrange("p (q c) -> p q c", c=128)
                petr([(o3[:, q, :], hT[:, 4 * b + q, tc * 128:(tc + 1) * 128].bitcast(F32), ident[:, :]) for q in range(4)],
                     ["hT.%d" % tc, "ident"], ["ps.%d" % (6 + b)])
                A(lambda e, tc=tc, b=b: e.copy(h2TM[:, tc, b * 512:(b + 1) * 512], ps[6 + b][:, :]), ["ps.%d" % (6 + b)], ["h2TM.%d.%d" % (tc, b)])
        p3 = ps[5][:, 0:256].rearrange("p (a c) -> p a c", c=16)
        petr([(p3[:, tc, :], posT[:, tc * 128:(tc + 1) * 128], ident[0:16, 0:16]) for tc in range(16)], ["posT", "ident"], ["ps.5"])
        V(lambda e: e.tensor_copy(posTM, ps[5][:, 0:256]), ["ps.5"], ["posTM"])
        p3b = ps[4][:, 0:256].rearrange("p (a c) -> p a c", c=16)
        petr([(p3b[:, tc, :], mgT[:, tc * 128:(tc + 1) * 128], ident[0:16, 0:16]) for tc in range(16)], ["mgT", "ident"], ["ps.4"])
        V(lambda e: e.tensor_copy(mgTM, ps[4][:, 0:256]), ["ps.4"], ["mgTM"])
        acc_w = RA[:, :].rearrange("p (a c) -> p a c", c=1024)
        acc = RA[:, :].bitcast(F32).rearrange("p (a c) -> p a c", c=1024)
        def acc_init():
            for tc in range(16):
                A(lambda e, tc=tc: e.mul(acc_w[:, tc, :], h2TM[:, tc, :].bitcast(F32), ALPHA),
                  ["h2TM.%d.0" % tc, "h2TM.%d.1" % tc] + tk("hT", 0, 16), ["acc.%d.0" % tc, "acc.%d.1" % tc])

        if stop == 5:
            acc_init()
        P.barrier()
        if stop == 5:
            P.add("pool", lambda e: e.nop(), [], ["DUMP"])
            dump_and_finish()
            P.emit(nc)
            return nc

        cd3 = Rb(0, 2048).rearrange("p (a c) -> p a c", c=1024)
        xs3 = Rb(2048, 2048).rearrange("p (j c) -> p j c", c=256)
        selp = [Rb(4096 + i * 256, 256) for i in range(2)]
        actT = [Rb(4608 + i * 256, 256) for i in range(2)]
        SelGh = [Rb(5120 + i * 512, 512).rearrange("p (a t) -> p a t", t=256) for i in range(2)]
        xst3 = Fb(0, 2048).rearrange("p (a c) -> p a c", c=1024)
        sgt2 = [Fb(4096, 256), Fb(4864, 256)]
        sgtm = [Fb(4352 + i * 256, 256) for i in range(2)]

        def scatter_items(ex):
            items = []
            for hb in range(8):
                SG = SelGh[hb % 2]
                sgk = "SelG%d" % (hb % 2)

                def prep(hb=hb, SG=SG, sgk=sgk):
                    for t in range(2):
                        tc = hb * 2 + t
                        sm = sgtm[t]
                        V(lambda e, sm=sm, tc=tc: e.tensor_scalar(sm, iotaf, posTM[:, tc * 16 + ex:tc * 16 + ex + 1], mgTM[:, tc * 16 + ex:tc * 16 + ex + 1], ALU.is_equal, ALU.mult),
                          ["iotaf", "posTM", "mgTM"], ["sgtm%d" % t])
                        petr([(ps[6][:, (cc * 2 + t) * 128:(cc * 2 + t + 1) * 128], sm[:, cc * 128:(cc + 1) * 128], ident[:, :]) for cc in range(2)],
                             ["sgtm%d" % t, "ident"], ["ps.6"])
                    copy_any(SG, ps[6][:, :].rearrange("p (a t) -> p a t", t=256), ["ps.6"], [sgk])
                items.append(prep)
                for t in range(2):
                    for db in range(2):
                        def tile(hb=hb, t=t, db=db, SG=SG, sgk=sgk):
                            tc = hb * 2 + t
                            pe([(ps[7][:, :], SG[:, cc, t * 128:(t + 1) * 128], cd3[:, cc, db * 512:(db + 1) * 512], cc == 0, cc == 1)
                                for cc in range(2)], [sgk, "cd.0.%d" % db, "cd.1.%d" % db], ["ps.7"])
                            V(lambda e: e.tensor_tensor(acc_w[:, tc, db * 512:(db + 1) * 512], acc[:, tc, db * 512:(db + 1) * 512], ps[7][:, :], ALU.add),
                              ["ps.7", "acc.%d.%d" % (tc, db)], ["acc.%d.%d" % (tc, db)])
                        items.append(tile)
            return items

        acc_init()
        pending = []

        def drain(n):
            for _ in range(n):
                if pending:
                    pending.pop(0)()

        for ex in range(16):
            for tc in range(16):
                sp_ = selp[tc % 2]
                V(lambda e, sp_=sp_, tc=tc, ex=ex: e.tensor_scalar(sp_, iotaf, posTM[:, tc * 16 + ex:tc * 16 + ex + 1], None, ALU.is_equal),
                  ["iotaf", "posTM"], ["selp%d" % (tc % 2)])
                pe([(ps[dch // 2][:, (dch % 2) * 256:(dch % 2 + 1) * 256], h2TM[:, tc, dch * 128:(dch + 1) * 128], sp_,
                     tc == 0 and dch % 2 == 0, tc == 15) for dch in range(8)],
                   ["selp%d" % (tc % 2), "h2TM.%d.0" % tc, "h2TM.%d.1" % tc], tk("ps", 0, 4), skip=True)
            for b4 in range(4):
                copy_any(xs3[:, 2 * b4:2 * b4 + 2, :], ps[b4][:, :].rearrange("p (a c) -> p a c", c=256), ["ps.%d" % b4], ["xsT.%d" % b4])

            def dma_down(fb, ex=ex):
                sset = fb % 2
                dma("pool", SR[:, 10240 + sset * 1024:10240 + (sset + 1) * 1024], wd_d[ex][fb * 128:(fb + 1) * 128, :], w=["WS%dd" % sset])

            def ffn_front(fb, ex=ex):
                sset = fb % 2
                o = 6144 + sset * 2048
                wgs = SR[:, o:o + 1024].rearrange("p (j c) -> p j c", c=128)
                wus = SR[:, o + 1024:o + 2048].rearrange("p (j c) -> p j c", c=128)
                wds = SR[:, 10240 + sset * 1024:10240 + (sset + 1) * 1024]
                wt = "WS%d" % sset
                dma("pool", SR[:, o:o + 1024], wg_d[ex, fb], w=[wt + "g"])
                dma("pool", SR[:, o + 1024:o + 2048], wu_d[ex, fb], w=[wt + "u"])
                if fb >= 1:
                    dma_down(fb - 1)
                pe([(ps[4][:, 0:256], wgs[:, j, :], xs3[:, j, :], j == 0, j == 7) for j in range(8)], [wt + "g"] + ["xsT.%d" % b_ for b_ in range(4)], ["ps.4"])
                drain(1)
                pe([(ps[5][:, 0:256], wus[:, j, :], xs3[:, j, :], j == 0, j == 7) for j in range(8)], [wt + "u"] + ["xsT.%d" % b_ for b_ in range(4)], ["ps.5"])
                sg_ = sgt2[sset]
                at_ = actT[sset]
                A(lambda e, sg_=sg_: e.activation(sg_, ps[4][:, 0:256], AF.Silu), ["ps.4"], ["sgt%d" % sset])
                V(lambda e, at_=at_, sg_=sg_: e.tensor_tensor(at_, sg_, ps[5][:, 0:256], ALU.mult),
                  ["sgt%d" % sset, "ps.5"], ["actT%d" % sset])
                drain(1)

            def ffn_down(fb):
                sset = fb % 2
                wds = SR[:, 10240 + sset * 1024:10240 + (sset + 1) * 1024]
                at_ = actT[sset]
                pe([(ps[cc * 2 + db][:, :], at_[:, cc * 128:(cc + 1) * 128], wds[:, db * 512:(db + 1) * 512], fb == 0, fb == 15)
                    for cc in range(2) for db in range(2)], ["actT%d" % sset, "WS%dd" % sset], tk("ps", 0, 4))
                drain(1)

            ffn_front(0)
            for fb in range(16):
                if fb + 1 < 16:
                    ffn_front(fb + 1)
                else:
                    dma_down(15)
                ffn_down(fb)
            drain(len(pending))
            for cc in range(2):
                for db in range(2):
                    copy_any(cd3[:, cc, db * 512:(db + 1) * 512], ps[cc * 2 + db][:, :], ["ps.%d" % (cc * 2 + db)], ["cd.%d.%d" % (cc, db)])
            pending = scatter_items(ex)
        drain(len(pending))
        load_ln(3)
        def ln3_chain(tc):
            par = tc % 2
            xb = xt[par]
            xk = "xt%d" % par
            yield from ln_chunk_gen(acc[:, tc, 0:512], acc[:, tc, 512:1024], ["acc.%d.0" % tc, "acc.%d.1" % tc], xb[:, :], [xk], xb[:, :], xk, par)
            dma("sp", out_d[tc * 128:(tc + 1) * 128, :], xb[:, :], r=[xk], outp=True)
            yield

        for tc in range(0, 16, 2):
            interleave(ln3_chain(tc), ln3_chain(tc + 1))
        P.emit(nc)
    return nc


def _consts():
    c = {}
    c["c_ident"] = np.eye(128, dtype=np.float32)
    c["c_aI"] = (np.eye(128) * ALPHA).astype(np.float32)
    a4 = np.zeros((128, 4, 512), np.float32)
    for j in range(4):
        a4[np.arange(128), j, j * 128 + np.arange(128)] = ALPHA
    c["c_aI4"] = np.ascontiguousarray(a4.reshape(128, 2048))
    c["c_ones"] = np.ones((128, 128), np.float32)
    s = np.arange(128)[:, None]
    cc = np.arange(128)[None, :]
    same = (s // 64) == (cc // 64)
    c["c_triF"] = (same & (s <= cc)).astype(np.float32)
    c["c_triB"] = (same & (s >= cc)).astype(np.float32)
    rm = np.ones((128, 512), np.float32)
    rm[:, ::64] = 0.0
    c["c_rmask"] = rm
    inv = 1.0 / (500000.0 ** (np.arange(0, 16, 2, dtype=np.float32) / 16.0))
    ang = np.arange(T, dtype=np.float32)[None, :] * inv[:, None]
    C = np.ones((128, T), np.float32)
    S = np.zeros((128, T), np.float32)
    for comp in range(2):
        b = comp * 64
        C[b:b + 8] = np.cos(ang)
        C[b + 8:b + 16] = np.cos(ang)
        S[b:b + 8] = -np.sin(ang)
        S[b + 8:b + 16] = np.sin(ang)
    c["c_ropeC"] = C
    c["c_ropeS"] = S
    c["c_iotaf"] = np.tile(np.arange(256, dtype=np.float32)[None, :], (128, 1))
    return c


def _swap_cols(w_in):
    out = np.empty((D, 1024), np.float32)
    for bi, base in enumerate((2560, 3072)):
        blk = w_in[:, base:base + 512].reshape(D, 8, 64)
        sw = blk.copy()
        sw[:, :, 0:8] = blk[:, :, 8:16]
        sw[:, :, 8:16] = blk[:, :, 0:8]
        out[:, bi * 512:(bi + 1) * 512] = sw.reshape(D, 512)
    return out


def _ffn_layout(w):
    e = w.shape[0]
    v = w.reshape(e, 8, 128, 16, 128).transpose(0, 3, 2, 1, 4)
    return np.ascontiguousarray(v).reshape(e, 16, 128, 1024)


_NC_CACHE = {}


def _prep_inputs(inp):
    f = lambda a: np.ascontiguousarray(np.asarray(a, dtype=np.float32))
    w_in = f(inp["w_in"])[0]
    shared = {
        "w_in": w_in,
        "w_sw": _swap_cols(w_in),
        "w_mix": f(inp["w_mix_out"])[0],
        "xa_wq": f(inp["xa_wq"])[0], "xa_wk": f(inp["xa_wk"])[0], "xa_wv": f(inp["xa_wv"])[0], "xa_wo": f(inp["xa_wo"])[0],
        "w_router": f(inp["w_router"])[0],
        "w_gate": _ffn_layout(f(inp["w_gate"])[0]), "w_up": _ffn_layout(f(inp["w_up"])[0]), "w_down": f(inp["w_down"])[0],
        "ln_g": np.stack([f(inp["emb_ln_g"]), f(inp["ln1_g"])[0], f(inp["ln2_g"])[0], f(inp["ln3_g"])[0]]),
        "ln_b": np.stack([f(inp["emb_ln_b"]), f(inp["ln1_b"])[0], f(inp["ln2_b"])[0], f(inp["ln3_b"])[0]]),
        "hg_lb": f(inp["hg_lb_logits"]).reshape(16, 128),
        "hg_norm_g": f(inp["hg_norm_g"]).reshape(4, 128),
        "da_subln_g": f(inp["da_subln_g"]).reshape(1, 128),
        "da_lam": np.stack([f(inp["da_lambda_q1"])[0], f(inp["da_lambda_k1"])[0], f(inp["da_lambda_q2"])[0], f(inp["da_lambda_k2"])[0]]),
    }
    shared.update(_consts())
    x = f(inp["x"])
    mem = f(inp["mem"])
    return [dict(shared, x=x[b], mem=mem[b]) for b in range(8)]


def kernel(**inputs):
    if "full" not in _NC_CACHE:
        _NC_CACHE["full"] = build(None)
    nc = _NC_CACHE["full"]
    in_maps = _prep_inputs(inputs)
    res = run_bass_kernel_spmd(nc, in_maps, core_ids=list(range(8)))
    return np.stack([np.asarray(r["out"], dtype=np.float32) for r in res.results], axis=0)
```
